# Optimizing a Trainium2 kernel written in Bass

```python
import jax, jax.numpy as jnp
from jax import lax
import numpy as np


D_MODEL = 1024
BATCH = 2
SEQ = 8192
DEPTH = 1

HEAD_DIM = 64
NSA_HEADS = 8
NSA_KV = 2
CMP_BLOCK = 32
CMP_STRIDE = 16
CMP_HIDDEN = 128
SEL_BLOCK = 64
SEL_TOPN = 16
NSA_WINDOW = 512
SWA_HEADS = 8
SWA_KV = 2
SWA_WINDOW = 128
Q_BLOCK = 128
ROPE_THETA = 10000.0
NEG = -1e30
N_EXPERTS = 32
TOP_K = 4
D_FF = 1024
SWIGLU_LIMIT = 7.0
SWIGLU_ALPHA = 1.702
MOE_BLOCK = 128
DN_ALPHA = (2.0 * DEPTH) ** 0.25
DN_BETA = (8.0 * DEPTH) ** -0.25
LN_EPS = 1e-5

NSA_Q = NSA_HEADS * HEAD_DIM
NSA_KVW = NSA_KV * HEAD_DIM
SWA_Q = SWA_HEADS * HEAD_DIM
SWA_KVW = SWA_KV * HEAD_DIM
IN_WIDTHS = (
    NSA_Q,
    NSA_KVW, NSA_KVW,
    NSA_KVW, NSA_KVW,
    NSA_KVW, NSA_KVW,
    NSA_HEADS * 3,
    SWA_Q, SWA_KVW, SWA_KVW,
    2 * D_MODEL,
)
IN_COLS = sum(IN_WIDTHS)
IN_SPLITS = tuple(sum(IN_WIDTHS[:i + 1]) for i in range(len(IN_WIDTHS) - 1))

kernel_name = 'hybrid_nsa_swasink_moe_deepnorm'


def layer_norm(x, g, b):
    xf = x.astype(jnp.float32)
    mu = jnp.mean(xf, -1, keepdims=True)
    var = jnp.mean(jnp.square(xf - mu), -1, keepdims=True)
    return ((xf - mu) * lax.rsqrt(var + LN_EPS) * g + b).astype(x.dtype)


def rope_tables(s):
    half = HEAD_DIM // 2
    inv = ROPE_THETA ** (-jnp.arange(half, dtype=jnp.float32) / half)
    ang = jnp.arange(s, dtype=jnp.float32)[:, None] * inv[None, :]
    return jnp.cos(ang), jnp.sin(ang)


def rope(t, cos, sin):
    half = HEAD_DIM // 2
    tf = t.astype(jnp.float32)
    t1, t2 = tf[..., :half], tf[..., half:]
    return jnp.concatenate([t1 * cos - t2 * sin, t2 * cos + t1 * sin], -1).astype(t.dtype)


def to_heads(t, n):
    b, s, _ = t.shape
    return t.reshape(b, s, n, HEAD_DIM).transpose(0, 2, 1, 3)


def from_heads(t):
    b, h, s, d = t.shape
    return t.transpose(0, 2, 1, 3).reshape(b, s, h * d)


def banded_attention(q, k, v, window, sinks=None):
    b, h, s, dh = q.shape
    g = k.shape[1]
    r = h // g
    nb = s // Q_BLOCK
    back = -(-window // Q_BLOCK)
    wlen = (back + 1) * Q_BLOCK
    qb = q.reshape(b, g, r, nb, Q_BLOCK, dh)

    def windows(t):
        tp = jnp.pad(t, ((0, 0), (0, 0), (back * Q_BLOCK, 0), (0, 0))).reshape(b, g, nb + back, Q_BLOCK, dh)
        return jnp.concatenate([tp[:, :, j:j + nb] for j in range(back + 1)], axis=3)

    kw, vw = windows(k), windows(v)
    qpos = jnp.arange(s).reshape(nb, Q_BLOCK)
    kpos = (jnp.arange(nb)[:, None] - back) * Q_BLOCK + jnp.arange(wlen)[None, :]
    rel = qpos[:, :, None] - kpos[:, None, :]
    mask = (rel >= 0) & (rel < window) & (kpos[:, None, :] >= 0)
    sc = jnp.einsum('bgrnqd,bgnkd->bgrnqk', qb, kw, preferred_element_type=jnp.float32) * (dh ** -0.5)
    sc = jnp.where(mask, sc, NEG)
    if sinks is None:
        p = jax.nn.softmax(sc, axis=-1)
    else:
        sk = sinks.astype(jnp.float32).reshape(1, g, r, 1, 1, 1)
        m = jnp.maximum(jnp.max(sc, -1, keepdims=True), sk)
        e = jnp.exp(sc - m)
        p = e / (jnp.sum(e, -1, keepdims=True) + jnp.exp(sk - m))
    o = jnp.einsum('bgrnqk,bgnkd->bgrnqd', p.astype(v.dtype), vw)
    return o.reshape(b, h, s, dh)


def nsa_compress(t, pe, w1, w2):
    s = t.shape[2]
    nc = (s - CMP_BLOCK) // CMP_STRIDE + 1
    idx = jnp.arange(nc)[:, None] * CMP_STRIDE + jnp.arange(CMP_BLOCK)[None, :]
    blk = t[:, :, idx] + pe
    flat = blk.reshape(blk.shape[0], blk.shape[1], nc, CMP_BLOCK * HEAD_DIM)
    return jax.nn.gelu(flat @ w1) @ w2


def nsa_attention(q, kc_raw, vc_raw, k_sel, v_sel, k_win, v_win, gate, cos, sin,
                  k_pe, k_w1, k_w2, v_pe, v_w1, v_w2):
    b, h, s, dh = q.shape
    g = NSA_KV
    r = h // g
    scale = dh ** -0.5
    pos = jnp.arange(s)
    kc = nsa_compress(kc_raw, k_pe, k_w1, k_w2)
    vc = nsa_compress(vc_raw, v_pe, v_w1, v_w2)
    nc = kc.shape[2]
    qg = q.reshape(b, g, r, s, dh)
    sc = jnp.einsum('bgrsd,bgcd->bgrsc', qg, kc, preferred_element_type=jnp.float32) * scale
    cend = jnp.arange(nc) * CMP_STRIDE + CMP_BLOCK - 1
    cmask = cend[None, :] <= pos[:, None]
    p_cmp = jax.nn.softmax(jnp.where(cmask, sc, NEG), axis=-1) * (pos >= CMP_BLOCK - 1)[:, None]
    o_cmp = jnp.einsum('bgrsc,bgcd->bgrsd', p_cmp.astype(vc.dtype), vc).reshape(b, h, s, dh)
    nsel = s // SEL_BLOCK
    cstart = jnp.arange(nc) * CMP_STRIDE
    sstart = jnp.arange(nsel) * SEL_BLOCK
    overlap = ((cstart[:, None] < sstart[None, :] + SEL_BLOCK) &
               (cstart[:, None] + CMP_BLOCK > sstart[None, :])).astype(jnp.float32)
    imp = jnp.einsum('bgrsc,cj->bgsj', p_cmp, overlap)
    cur = pos // SEL_BLOCK
    jb = jnp.arange(nsel)
    forced = (jb[None, :] == 0) | (jb[None, :] == cur[:, None]) | (jb[None, :] == cur[:, None] - 1)
    imp = jnp.where(jb[None, :] > cur[:, None], -1.0, jnp.where(forced, 1e6, imp))
    n_top = min(SEL_TOPN, nsel)
    _, sel = lax.top_k(imp, n_top)
    q_rot = rope(q, cos, sin)
    kb = rope(k_sel, cos, sin).reshape(b, g, nsel, SEL_BLOCK, dh)
    vb = v_sel.reshape(b, g, nsel, SEL_BLOCK, dh)
    nq = s // Q_BLOCK
    qr = q_rot.reshape(b, g, r, nq, Q_BLOCK, dh).transpose(3, 0, 1, 2, 4, 5)
    selr = sel.reshape(b, g, nq, Q_BLOCK, n_top).transpose(2, 0, 1, 3, 4)
    posr = pos.reshape(nq, Q_BLOCK)
    bi = jnp.arange(b)[:, None, None, None]
    gi = jnp.arange(g)[None, :, None, None]
    offs = jnp.arange(SEL_BLOCK)

    def sel_chunk(args):
        qc, sc_idx, pc = args
        ks = kb[bi, gi, sc_idx]
        vs = vb[bi, gi, sc_idx]
        kp = sc_idx[..., None] * SEL_BLOCK + offs
        msk = kp <= pc[None, None, :, None, None]
        ss = jnp.einsum('bgrqd,bgqnkd->bgrqnk', qc, ks, preferred_element_type=jnp.float32) * scale
        ss = jnp.where(msk[:, :, None], ss, NEG)
        p = jax.nn.softmax(ss, axis=(-2, -1))
        return jnp.einsum('bgrqnk,bgqnkd->bgrqd', p.astype(vs.dtype), vs)

    o_sel = lax.map(sel_chunk, (qr, selr, posr))
    o_sel = o_sel.transpose(1, 2, 3, 0, 4, 5).reshape(b, h, s, dh)
    o_win = banded_attention(q_rot, rope(k_win, cos, sin), v_win, NSA_WINDOW)
    gt = jax.nn.sigmoid(gate.astype(jnp.float32)).reshape(b, s, h, 3).transpose(0, 2, 1, 3)[..., None]
    o = gt[:, :, :, 0] * o_cmp + gt[:, :, :, 1] * o_sel + gt[:, :, :, 2] * o_win
    return o.astype(q.dtype)


def moe_ffn(x, w_router, b_router, w_e_in, b_e_in, w_e_out, b_e_out):
    b, s, d = x.shape
    t = b * s
    a = t * TOP_K
    xf = x.reshape(t, d)
    logits = (xf @ w_router).astype(jnp.float32) + b_router
    top_v, top_e = lax.top_k(logits, TOP_K)
    gates = jax.nn.softmax(top_v, axis=-1)
    flat_e = top_e.reshape(a)
    flat_t = jnp.arange(a, dtype=jnp.int32) // TOP_K
    flat_g = gates.reshape(a)
    order = jnp.argsort(flat_e)
    se = flat_e[order]
    counts = jnp.bincount(flat_e, length=N_EXPERTS)
    padded = (counts + MOE_BLOCK - 1) // MOE_BLOCK * MOE_BLOCK
    start = jnp.cumsum(counts) - counts
    pend = jnp.cumsum(padded)
    pstart = pend - padded
    dest = pstart[se] + jnp.arange(a) - start[se]
    n_blk = -(-a // MOE_BLOCK) + N_EXPERTS
    n_rows = n_blk * MOE_BLOCK
    row_t = jnp.zeros((n_rows,), jnp.int32).at[dest].set(flat_t[order])
    row_g = jnp.zeros((n_rows,), jnp.float32).at[dest].set(flat_g[order])
    blk_e = jnp.minimum(jnp.searchsorted(pend, jnp.arange(n_blk) * MOE_BLOCK, side='right'), N_EXPERTS - 1)
    xs = xf[row_t].reshape(n_blk, MOE_BLOCK, d)

    def expert_block(args):
        xb, e = args
        hdn = xb @ w_e_in[e] + b_e_in[e]
        hg = jnp.minimum(hdn[:, :D_FF], SWIGLU_LIMIT)
        hu = jnp.clip(hdn[:, D_FF:], -SWIGLU_LIMIT, SWIGLU_LIMIT)
        act = hg * jax.nn.sigmoid(SWIGLU_ALPHA * hg) * (hu + 1.0)
        return act @ w_e_out[e] + b_e_out[e]

    ys = lax.map(expert_block, (xs, blk_e)).reshape(n_rows, d)
    out = jax.ops.segment_sum(ys.astype(jnp.float32) * row_g[:, None], row_t, num_segments=t)
    return out.reshape(b, s, d).astype(x.dtype)


def hybrid_layer(x, cos, sin, w_in, nsa_k_pe, nsa_k_w1, nsa_k_w2, nsa_v_pe, nsa_v_w1, nsa_v_w2,
                 swa_sinks, w_br_nsa, w_br_swa, w_out, ln1_g, ln1_b, w_router, b_router,
                 w_expert_in, b_expert_in, w_expert_out, b_expert_out, ln2_g, ln2_b):
    d = x.shape[-1]
    proj = x @ w_in
    (q_n, kc_n, vc_n, ks_n, vs_n, kw_n, vw_n, g_n, q_s, k_s, v_s, g_m) = jnp.split(proj, IN_SPLITS, axis=-1)
    o_nsa = nsa_attention(to_heads(q_n, NSA_HEADS), to_heads(kc_n, NSA_KV), to_heads(vc_n, NSA_KV),
                          to_heads(ks_n, NSA_KV), to_heads(vs_n, NSA_KV), to_heads(kw_n, NSA_KV),
                          to_heads(vw_n, NSA_KV), g_n, cos, sin,
                          nsa_k_pe, nsa_k_w1, nsa_k_w2, nsa_v_pe, nsa_v_w1, nsa_v_w2)
    o_swa = banded_attention(rope(to_heads(q_s, SWA_HEADS), cos, sin), rope(to_heads(k_s, SWA_KV), cos, sin),
                             to_heads(v_s, SWA_KV), SWA_WINDOW, swa_sinks)
    y_nsa = from_heads(o_nsa) @ w_br_nsa
    y_swa = from_heads(o_swa) @ w_br_swa
    gm = jax.nn.sigmoid(g_m.astype(jnp.float32))
    mixed = (gm[..., :d] * y_nsa + gm[..., d:] * y_swa).astype(x.dtype)
    h = layer_norm(DN_ALPHA * x + mixed @ w_out, ln1_g, ln1_b)
    f = moe_ffn(h, w_router, b_router, w_expert_in, b_expert_in, w_expert_out, b_expert_out)
    return layer_norm(DN_ALPHA * h + f, ln2_g, ln2_b)


def setup_inputs(seed: int = 0) -> dict:
    key = jax.random.key(seed)
    ks = jax.random.split(key, 24)
    L, D, E, F = DEPTH, D_MODEL, N_EXPERTS, D_FF
    cw = CMP_BLOCK * HEAD_DIM

    def nrm(k, shape, scale):
        return jax.random.normal(k, shape, jnp.float32) * scale

    return {
        'x': nrm(ks[0], (BATCH, SEQ, D), 1.0),
        'w_in': nrm(ks[1], (L, D, IN_COLS), D ** -0.5),
        'nsa_k_pe': nrm(ks[2], (L, CMP_BLOCK, HEAD_DIM), 0.1),
        'nsa_k_w1': nrm(ks[3], (L, cw, CMP_HIDDEN), cw ** -0.5),
        'nsa_k_w2': nrm(ks[4], (L, CMP_HIDDEN, HEAD_DIM), CMP_HIDDEN ** -0.5),
        'nsa_v_pe': nrm(ks[5], (L, CMP_BLOCK, HEAD_DIM), 0.1),
        'nsa_v_w1': nrm(ks[6], (L, cw, CMP_HIDDEN), cw ** -0.5),
        'nsa_v_w2': nrm(ks[7], (L, CMP_HIDDEN, HEAD_DIM), CMP_HIDDEN ** -0.5),
        'swa_sinks': nrm(ks[8], (L, SWA_HEADS), 0.5),
        'w_br_nsa': nrm(ks[9], (L, NSA_Q, D), NSA_Q ** -0.5),
        'w_br_swa': nrm(ks[10], (L, SWA_Q, D), SWA_Q ** -0.5),
        'w_out': nrm(ks[11], (L, D, D), DN_BETA * D ** -0.5),
        'ln1_g': 1.0 + nrm(ks[12], (L, D), 0.01),
        'ln1_b': nrm(ks[13], (L, D), 0.01),
        'w_router': nrm(ks[14], (L, D, E), D ** -0.5),
        'b_router': nrm(ks[15], (L, E), 0.01),
        'w_expert_in': nrm(ks[16], (L, E, D, 2 * F), D ** -0.5),
        'b_expert_in': nrm(ks[17], (L, E, 2 * F), 0.01),
        'w_expert_out': nrm(ks[18], (L, E, F, D), DN_BETA * F ** -0.5),
        'b_expert_out': nrm(ks[19], (L, E, D), 0.01),
        'ln2_g': 1.0 + nrm(ks[20], (L, D), 0.01),
        'ln2_b': nrm(ks[21], (L, D), 0.01),
    }


def reference(x, w_in, nsa_k_pe, nsa_k_w1, nsa_k_w2, nsa_v_pe, nsa_v_w1, nsa_v_w2, swa_sinks,
              w_br_nsa, w_br_swa, w_out, ln1_g, ln1_b, w_router, b_router, w_expert_in, b_expert_in,
              w_expert_out, b_expert_out, ln2_g, ln2_b):
    cos, sin = rope_tables(x.shape[1])
    for l in range(DEPTH):
        x = hybrid_layer(x, cos, sin, w_in[l], nsa_k_pe[l], nsa_k_w1[l], nsa_k_w2[l], nsa_v_pe[l],
                         nsa_v_w1[l], nsa_v_w2[l], swa_sinks[l], w_br_nsa[l], w_br_swa[l], w_out[l],
                         ln1_g[l], ln1_b[l], w_router[l], b_router[l], w_expert_in[l], b_expert_in[l],
                         w_expert_out[l], b_expert_out[l], ln2_g[l], ln2_b[l])
    return x
```

```python
import contextlib
import numpy as np
import ml_dtypes
import concourse.bass as bass
import concourse.mybir as mybir
from concourse.bass_utils import run_bass_kernel_spmd

F32 = mybir.dt.float32
BF16 = mybir.dt.bfloat16
I32 = mybir.dt.int32
AF = mybir.ActivationFunctionType
ALU = mybir.AluOpType
AX = mybir.AxisListType

S = 8192
DM = 1024
NJ = 16
CAP = 384
_LV = 9
NSLOT = 32 * CAP
ALPHA = 2.0 ** 0.25
EPS = 1e-5


class Sched:
    ENGS = ("pe", "act", "dve", "pool", "sp")
    NDMA = 24

    def __init__(self, nc, sems):
        self.nc = nc
        self.sems = sems
        self.ops = {e: [] for e in self.ENGS}
        self.seq = {e: 0 for e in self.ENGS}
        self.waited = {e: {} for e in self.ENGS}
        self.last_w = {}
        self.readers = {}
        self.dma_i = 0
        self.dma_tok = [None] * self.NDMA
        self.dma_val = [0] * self.NDMA
        self.out_toks = []
        self.enabled = True

    def _deps(self, reads, writes, eng=None):
        deps = []
        for r in reads:
            t = self.last_w.get(r)
            if t is not None:
                deps.append(t)
            if r.startswith("pb"):
                deps.extend(x for x in self.readers.get(r, ()) if x[0] != eng)
        for w in writes:
            t = self.last_w.get(w)
            if t is not None:
                deps.append(t)
            deps.extend(self.readers.get(w, ()))
        return deps

    def _commit(self, tok, reads, writes):
        for r in reads:
            self.readers.setdefault(r, []).append(tok)
        for w in writes:
            self.last_w[w] = tok
            self.readers[w] = []

    def _waits(self, eng, deps):
        need = {}
        for (k, v) in deps:
            if k == eng and eng == "pe":
                continue
            if self.waited[eng].get(k, 0) >= v:
                continue
            if need.get(k, 0) < v:
                need[k] = v
        for k, v in need.items():
            self.waited[eng][k] = v
        return list(need.items())

    def op(self, eng, fn, reads=(), writes=()):
        if not self.enabled:
            return None
        waits = self._waits(eng, self._deps(reads, writes, eng))
        self.seq[eng] += 1
        tok = (eng, self.seq[eng])
        self.ops[eng].append((waits, fn, (eng, 1)))
        self._commit(tok, reads, writes)
        return tok

    def dma(self, fn, reads=(), writes=(), q="sp"):
        if not self.enabled:
            return None
        i = self.dma_i % self.NDMA
        self.dma_i += 1
        deps = self._deps(reads, writes, q)
        if self.dma_tok[i] is not None:
            deps.append(self.dma_tok[i])
        waits = self._waits(q, deps)
        self.dma_val[i] += 16
        tok = (("dma", i), self.dma_val[i])
        self.dma_tok[i] = tok
        self.ops[q].append((waits, fn, (("dma", i), 16)))
        self._commit(tok, reads, writes)
        return tok

    def barrier(self):
        toks = [(e, self.seq[e]) for e in self.ENGS if self.seq[e] > 0]
        toks += [(("dma", i), self.dma_val[i]) for i in range(self.NDMA) if self.dma_val[i] > 0]
        for eng in self.ENGS:
            waits = self._waits(eng, toks)
            if waits:
                self.ops[eng].append((waits, None, None))

    def emit(self, final=False):
        nc = self.nc
        sems = self.sems
        self.barrier()
        if final:
            waits = self._waits("sp", list(self.out_toks))
            self.ops["sp"].append((waits, None, None))
        with nc.Block() as block:
            def run(engobj, name):
                for waits, fn, inc in self.ops[name]:
                    for k, v in waits:
                        engobj.wait_ge(sems[k], v)
                    if fn is None:
                        continue
                    ins = fn(engobj)
                    ins.then_inc(sems[inc[0]], inc[1])
                self.ops[name] = []

            @block.tensor
            def _(e):
                run(e, "pe")

            @block.scalar
            def _(e):
                run(e, "act")

            @block.vector
            def _(e):
                run(e, "dve")

            @block.gpsimd
            def _(e):
                run(e, "pool")

            @block.sync
            def _(e):
                run(e, "sp")


def build_nc():
    nc = bass.Bass("TRN2", target_bir_lowering=False)

    def DR(name, shape, dt=F32, kind="ExternalInput"):
        return nc.dram_tensor(name, shape, dt, kind=kind).ap()

    xTf = DR("xTf", [DM, S]); xTo = DR("xTo", [DM, 2048]); xo = DR("xo", [2048, DM])
    w_kvf = DR("w_kvf", [DM, 640]); w_vt = DR("w_vt", [DM, 384]); w_q = DR("w_q", [DM, 1024])
    w_gn = DR("w_gn", [DM, 24]); w_gm = DR("w_gm", [DM, 2048])
    ropef = DR("ropef", [128, 2, S]); ropeo = DR("ropeo", [128, NJ, 2, 512])
    pe2 = DR("pe2", [128, 2, 16]); cw1 = DR("cw1", [2, 2048, 128]); cw2 = DR("cw2", [128, 2, 64])
    cmaskT = DR("cmaskT", [128, NJ, 4, 512], BF16); AB = DR("AB", [128, NJ, 2, 128])
    validm = DR("validm", [128, 128]); ovl = DR("ovl", [128, 4, 128], BF16); indD = DR("ind", [64, S], BF16)
    constsD = DR("consts", [128, 9, 128], BF16); mb4D = DR("mb4", [128, 6, 512], BF16); identfD = DR("identf", [128, 128])
    sinksD = DR("sinks", [128, 8]); wbr = DR("wbr", [2, 512, DM]); wout = DR("wout", [DM, DM])
    lnD = DR("ln", [128, 4, DM]); wrD = DR("wr", [DM, 32]); brD = DR("br", [128, 32]); ecapD = DR("ecap", [128, 32])
    wei = DR("wei", [32, DM, 2048]); beiD = DR("bei", [128, 32, 16]); weo = DR("weo", [32, DM, DM]); beoD = DR("beo", [32, DM])
    yout = DR("y", [2048, DM], F32, "ExternalOutput")
    gmb = DR("gmb", [2048, 2048], BF16, "Internal")
    hbuf = DR("hbuf", [2048, DM], F32, "Internal")
    Xbuf = DR("Xbuf", [NSLOT + 128, DM], BF16, "Internal")
    Ybuf = DR("Ybuf", [NSLOT + 128, DM], F32, "Internal")
    obuf = DR("obuf", [2048, DM], BF16, "Internal")

    with contextlib.ExitStack() as top:
        sems = {}
        for e in Sched.ENGS:
            sems[e] = top.enter_context(nc.semaphore("s_" + e))
        for i in range(Sched.NDMA):
            sems[("dma", i)] = top.enter_context(nc.semaphore("s_dma%d" % i))
        s = Sched(nc, sems)

        def TT(stack, name, shape, dt):
            return stack.enter_context(nc.sbuf_tensor("sb_" + name, shape, dt))

        pb = [top.enter_context(nc.psum_tensor("pb%d" % i, [128, 512], F32)) for i in range(8)]
        PB = ["pb%d" % i for i in range(8)]

        def mm(out, lhsT, rhs, start, stop, reads, writes):
            s.op("pe", lambda e: e.matmul(out, lhsT=lhsT, rhs=rhs, start=start, stop=stop, skip_group_check=True), reads, writes)

        def tr(out, in_, ident, reads, writes):
            s.op("pe", lambda e: e.transpose(out, in_, ident), reads, writes)

        def cp(eng, out, in_, reads, writes):
            if eng == "act":
                s.op("act", lambda e: e.copy(out, in_), reads, writes)
            else:
                s.op(eng, lambda e: e.tensor_copy(out, in_), reads, writes)

        def tt(eng, out, in0, in1, op, reads, writes):
            s.op(eng, lambda e: e.tensor_tensor(out, in0, in1, op), reads, writes)

        def ts(eng, out, in0, s1, op0, reads, writes, s2=None, op1=None):
            if op1 is None:
                s.op(eng, lambda e: e.tensor_scalar(out, in0, s1, None, op0), reads, writes)
            else:
                s.op(eng, lambda e: e.tensor_scalar(out, in0, s1, s2, op0, op1), reads, writes)

        def stt(eng, out, in0, sc, in1, op0, op1, reads, writes):
            s.op(eng, lambda e: e.scalar_tensor_tensor(out, in0, sc, in1, op0, op1), reads, writes)

        def act(out, in_, func, reads, writes, bias=0.0, scale=1.0):
            s.op("act", lambda e: e.activation(out, in_, func, bias=bias, scale=scale), reads, writes)

        def dma(out, in_, reads, writes, q="sp"):
            return s.dma(lambda e: e.dma_start(out=out, in_=in_), reads, writes, q=q)

        consts = TT(top, "consts", [128, 9, 128], BF16)
        identf = TT(top, "identf", [128, 128], F32)
        stg = [TT(top, "stg%d" % i, [128, 2048], F32) for i in range(3)]
        nstg = {"n": 3}
        dma(consts[:], constsD, [], ["consts"])
        dma(identf[:], identfD, [], ["identf"])
        C_CAUSAL, C_ANTI, C_W0, C_W1, C_W2, C_S2, C_TRIU, C_ONES, C_ID = range(9)
        ident_bf = consts[:, C_ID, :]
        st_state = {"i": 0, "c": 0}
        cast_engs = ["pool", "act", "dve", "pool", "act"]

        def load_cast(dst, src, nelem, view, dkey, eng=None):
            i = st_state["i"] % nstg["n"]
            st_state["i"] += 1
            sv = view(stg[i][:, 0:nelem])
            dma(sv, src, [], ["stg%d" % i])
            if eng is None:
                eng = cast_engs[st_state["c"] % len(cast_engs)]
                st_state["c"] += 1
            cp(eng, dst, sv, ["stg%d" % i], [dkey])

        def lc_dma(src, nelem, view):
            i = st_state["i"] % nstg["n"]
            st_state["i"] += 1
            sv = view(stg[i][:, 0:nelem])
            dma(sv, src, [], ["stg%d" % i])
            return (sv, i)

        def lc_cast(h, dst, dkey, eng):
            cp(eng, dst, h[0], ["stg%d" % h[1]], [dkey])

        with contextlib.ExitStack() as ab:
            KI = [TT(ab, "KI%d" % g, [128, S], BF16) for g in range(2)]
            kwT = TT(ab, "kwT", [128, S], BF16)
            kswT = TT(ab, "kswT", [128, S // 2], BF16)
            vs_aug = TT(ab, "vs_aug", [128, 64, 2, 65], BF16)
            vw_aug = TT(ab, "vw_aug", [128, 64, 2, 65], BF16)
            vsw_aug = TT(ab, "vsw_aug", [128, 32, 2, 65], BF16)
            kcT = TT(ab, "kcT", [128, 512], BF16)
            vc_aug = TT(ab, "vc_aug", [128, 4, 2, 193], BF16)

            xTo_v = xTo.rearrange("(dc p) t -> p dc t", p=128)

            def load_xoj(dst, j, key):
                load_cast(dst[:, :, :], xTo_v[:, :, j * 128:(j + 1) * 128], 1024,
                          lambda a: a.rearrange("p (a b) -> p a b", a=8), key)

            def xoj_dma(j):
                return lc_dma(xTo_v[:, :, j * 128:(j + 1) * 128], 1024, lambda a: a.rearrange("p (a b) -> p a b", a=8))

            s.enabled = False
            with contextlib.ExitStack() as ph:
                wgm_bf = TT(ph, "wgm_bf", [128, 8, 2048], BF16)
                sg = [TT(ph, "sg%d" % i, [128, 2048], BF16) for i in range(2)]
                wgm_v = w_gm.rearrange("(dc p) c -> p dc c", p=128)
                xoj = [TT(ph, "xojA%d" % i, [128, 8, 128], BF16) for i in range(2)]
                for i in range(8):
                    load_cast(wgm_bf[:, i, :], wgm_v[:, i, :], 2048, lambda a: a, "wgm_bf")
                load_xoj(xoj[0], 0, "xoj0")
                for j in range(NJ):
                    sl = j % 2
                    xh = xoj_dma(j + 1) if j + 1 < NJ else None
                    for cc in range(4):
                        if cc == 2 and xh is not None:
                            lc_cast(xh, xoj[1 - sl][:, :, :], "xoj%d" % (1 - sl), "dve")
                        b = cc % 4
                        for dc in range(8):
                            mm(pb[b][:, :], xoj[sl][:, dc, :], wgm_bf[:, dc, cc * 512:(cc + 1) * 512],
                               dc == 0, dc == 7, ["xoj%d" % sl, "wgm_bf"], [PB[b]])
                        act(sg[sl][:, cc * 512:(cc + 1) * 512], pb[b][:, :], AF.Sigmoid, [PB[b]], ["sg%d_%d" % (sl, cc)])
                    dma(gmb[j * 128:(j + 1) * 128, :], sg[sl][:], ["sg%d_%d" % (sl, c) for c in range(4)], ["gmb%d" % j])
                s.emit()

            s.enabled = _LV >= 1
            with contextlib.ExitStack() as ph:
                wkvf_bf = TT(ph, "wkvf_bf", [128, 8, 640], BF16)
                wvt_bf = TT(ph, "wvt_bf", [128, 8, 384], BF16)
                cw1_bf = TT(ph, "cw1_bf", [128, 2, 16, 128], BF16)
                cw2_bf = TT(ph, "cw2_bf", [128, 2, 64], BF16)
                pe_bf = TT(ph, "pe_bf", [128, 2, 16], BF16)
                cbias = TT(ph, "cbias", [128, 2], F32)
                xt_bf = [TT(ph, "xt_bf%d" % i, [128, 8, 512], BF16) for i in range(2)]
                ropet = [TT(ph, "ropet%d" % i, [128, 2, 512], F32) for i in range(2)]
                cb = [[TT(ph, "cb%d_%d" % (i, k), [128, 528], BF16) for k in range(4)] for i in range(2)]
                tmpA = [TT(ph, "tmpA%d" % i, [128, 512], F32) for i in range(2)]
                tmpB = [TT(ph, "tmpB%d" % i, [128, 512], F32) for i in range(2)]
                gx = TT(ph, "gx", [128, 128], F32)
                gy = TT(ph, "gy", [128, 128], F32)
                gz = TT(ph, "gz", [128, 128], F32)
                gT = TT(ph, "gT", [128, 128], BF16)

                wk_v = w_kvf.rearrange("(dc p) c -> p dc c", p=128)
                for i in range(4):
                    load_cast(wkvf_bf[:, 2 * i:2 * i + 2, :], wk_v[:, 2 * i:2 * i + 2, :], 1280,
                              lambda a: a.rearrange("p (a b) -> p a b", a=2), "wkvf_bf")
                wvt_v = w_vt.rearrange("(dc p) c -> p dc c", p=128)
                for i in range(2):
                    load_cast(wvt_bf[:, 4 * i:4 * i + 4, :], wvt_v[:, 4 * i:4 * i + 4, :], 1536,
                              lambda a: a.rearrange("p (a b) -> p a b", a=4), "wvt_bf")
                cw1_v = cw1.rearrange("a (lp p) m -> p a lp m", p=128)
                for a_ in range(2):
                    load_cast(cw1_bf[:, a_, :, :], cw1_v[:, a_, :, :], 2048,
                              lambda a: a.rearrange("p (a b) -> p a b", a=16), "cw1_bf")
                load_cast(cw2_bf[:], cw2, 128, lambda a: a.rearrange("p (a b) -> p a b", a=2), "cw2_bf")
                load_cast(pe_bf[:], pe2, 32, lambda a: a.rearrange("p (a b) -> p a b", a=2), "pe_bf")
                for nm, vt in (("vs_aug", vs_aug), ("vw_aug", vw_aug), ("vsw_aug", vsw_aug)):
                    s.op("pool", (lambda t: (lambda e: e.memset(t[:, :, :, 64:65], 1.0)))(vt), [], [nm + "_one"])
                s.op("pool", lambda e: e.memset(vc_aug[:, :, :, 64:65], 1.0), [], ["vc_one"])
                for g in range(2):
                    dma(vc_aug[:, :, g, 65:193], ovl, [], ["vc_ovl%d" % g])
                for i in range(2):
                    for k in range(4):
                        s.op("pool", (lambda t: (lambda e: e.memset(t[:, :], 0.0)))(cb[i][k]), [], ["cb%d_%d" % (i, k)])
                for g in range(2):
                    dma(KI[g][64:128, :], indD, [], ["KIind%d" % g])
                for a_ in range(2):
                    for lp in range(16):
                        mm(pb[6][:, a_:a_ + 1], cw1_bf[:, a_, lp, :], pe_bf[:, a_, lp:lp + 1], lp == 0, lp == 15,
                           ["cw1_bf", "pe_bf"], [PB[6]])
                cp("dve", cbias[:, :], pb[6][:, 0:2], [PB[6]], ["cbias"])

                xTf_v = xTf.rearrange("(dc p) t -> p dc t", p=128)

                def rope_to(dst, ps, tab, ti, n, rkeys, wkeys, psk, split=None):
                    tt("dve", tmpA[ti][:, 0:n], ps, tab[:, 0, :], ALU.mult, [psk] + rkeys, ["tmpA%d" % ti])
                    for (o0, i0) in ((0, 32), (32, 0), (64, 96), (96, 64)):
                        tt("dve", tmpB[ti][o0:o0 + 32, 0:n], ps[i0:i0 + 32, :], tab[o0:o0 + 32, 1, :], ALU.mult,
                           [psk] + rkeys, ["tmpB%d_%d" % (ti, o0)])
                    if split is not None:
                        for g_ in range(2):
                            tt("pool", split[g_], tmpA[ti][64 * g_:64 * g_ + 64, 0:n], tmpB[ti][64 * g_:64 * g_ + 64, 0:n], ALU.add,
                               ["tmpA%d" % ti] + ["tmpB%d_%d" % (ti, o) for o in (0, 32, 64, 96)], wkeys)
                        return
                    tt("pool", dst, tmpA[ti][:, 0:n], tmpB[ti][:, 0:n], ALU.add,
                       ["tmpA%d" % ti] + ["tmpB%d_%d" % (ti, o) for o in (0, 32, 64, 96)], wkeys)

                def xt_dma(ti_):
                    return [lc_dma(xTf_v[:, 4 * hf:4 * hf + 4, ti_ * 512:(ti_ + 1) * 512], 2048,
                                   lambda a: a.rearrange("p (a b) -> p a b", a=4)) for hf in range(2)]

                def xt_cast(hs_, sl_):
                    for hf in range(2):
                        lc_cast(hs_[hf], xt_bf[sl_][:, 4 * hf:4 * hf + 4, :], "xt_bf%d" % sl_, "act" if hf == 0 else "dve")

                xt_cast(xt_dma(0), 0)
                dma(ropet[0][:], ropef[:, :, 0:512], [], ["ropet0"])
                for tt_i in range(16):
                    sl = tt_i % 2
                    t0 = tt_i * 512
                    xth = None
                    if tt_i + 1 < 16:
                        xth = xt_dma(tt_i + 1)
                        dma(ropet[1 - sl][:], ropef[:, :, t0 + 512:t0 + 1024], [], ["ropet%d" % (1 - sl)])
                    for fc in range(5):
                        if fc == 3 and xth is not None:
                            xt_cast(xth, 1 - sl)
                        b = fc % 2
                        for dc in range(8):
                            mm(pb[b][:, :], wkvf_bf[:, dc, fc * 128:(fc + 1) * 128], xt_bf[sl][:, dc, :], dc == 0, dc == 7,
                               ["wkvf_bf", "xt_bf%d" % sl], [PB[b]])
                        if fc < 2:
                            for g in range(2):
                                k = fc * 2 + g
                                ck = "cb%d_%d" % (sl, k)
                                cp("act", cb[sl][k][0:64, 16:528], pb[b][64 * g:64 * g + 64, :], [PB[b]], [ck])
                                cp("dve", cb[sl][k][64:128, 15:527], pb[b][64 * g:64 * g + 64, :], [PB[b]], [ck])
                        else:
                            if fc == 2:
                                rope_to(None, pb[b][:, :], ropet[sl], fc % 2, 512, ["ropet%d" % sl], ["KI"], PB[b],
                                        split=[KI[0][0:64, t0:t0 + 512], KI[1][0:64, t0:t0 + 512]])
                            elif fc == 3:
                                rope_to(kwT[:, t0:t0 + 512], pb[b][:, :], ropet[sl], fc % 2, 512, ["ropet%d" % sl], ["kwT"], PB[b])
                            else:
                                rope_to(kswT[:, tt_i * 256:(tt_i + 1) * 256], pb[b][:, 256:512], ropet[sl][:, :, 256:512], fc % 2, 256,
                                        ["ropet%d" % sl], ["kswT"], PB[b])
                    for ti in range(4):
                        b = 2 + ti % 2
                        for dc in range(8):
                            mm(pb[b][:, 0:384], xt_bf[sl][:, dc, ti * 128:(ti + 1) * 128], wvt_bf[:, dc, :], dc == 0, dc == 7,
                               ["xt_bf%d" % sl, "wvt_bf"], [PB[b]])
                        for vi, (nm, vt) in enumerate((("vs_aug", vs_aug), ("vw_aug", vw_aug), ("vsw_aug", vsw_aug))):
                            if vi == 2 and ti < 2:
                                continue
                            tix = (2 * tt_i + ti - 2) if vi == 2 else (4 * tt_i + ti)
                            cp("act", vt[:, tix, :, 0:64],
                               pb[b][:, vi * 128:(vi + 1) * 128].rearrange("p (g d) -> p g d", g=2), [PB[b]], [nm])
                    if tt_i > 0:
                        for k in range(4):
                            ck = "cb%d_%d" % (sl, k)
                            pk = "cb%d_%d" % (1 - sl, k)
                            cp("pool", cb[sl][k][0:64, 0:16], cb[1 - sl][k][0:64, 512:528], [pk], [ck])
                            cp("pool", cb[sl][k][64:128, 0:15], cb[1 - sl][k][64:128, 512:527], [pk], [ck])
                    for k in range(4):
                        a_ = k // 2
                        for lp in range(16):
                            mm(pb[4][:, k * 32:(k + 1) * 32], cw1_bf[:, a_, lp, :], cb[sl][k][:, 2 * lp:2 * lp + 497:16],
                               lp == 0, lp == 15, ["cw1_bf", "cb%d_%d" % (sl, k)], [PB[4]])
                    for a_ in range(2):
                        act(gx[:, a_ * 64:(a_ + 1) * 64], pb[4][:, a_ * 64:(a_ + 1) * 64], AF.Identity, [PB[4], "cbias"], ["gx%d" % a_],
                            bias=cbias[:, a_:a_ + 1])
                    tt("dve", gy[:, :], gx[:, :], gx[:, :], ALU.mult, ["gx0", "gx1"], ["gy"])
                    ts("dve", gy[:, :], gy[:, :], 0.044715, ALU.mult, ["gy"], ["gy"], 1.0, ALU.add)
                    tt("dve", gy[:, :], gy[:, :], gx[:, :], ALU.mult, ["gy", "gx0", "gx1"], ["gy"])
                    act(gz[:, :], gy[:, :], AF.Sigmoid, ["gy"], ["gz"], scale=1.5957691216057308)
                    tt("dve", gT[:, :], gz[:, :], gx[:, :], ALU.mult, ["gz", "gx0", "gx1"], ["gT"])
                    for g in range(2):
                        mm(pb[5][0:64, g * 32:(g + 1) * 32], cw2_bf[:, 0, :], gT[:, g * 32:(g + 1) * 32], True, True,
                           ["cw2_bf", "gT"], [PB[5]])
                    for g in range(2):
                        mm(pb[5][0:32, 64 + g * 64:128 + g * 64], gT[:, 64 + g * 32:96 + g * 32], cw2_bf[:, 1, :], True, True,
                           ["cw2_bf", "gT"], [PB[5]])
                    for g in range(2):
                        cp("dve", kcT[64 * g:64 * g + 64, tt_i * 32:(tt_i + 1) * 32], pb[5][0:64, g * 32:(g + 1) * 32], [PB[5]], ["kcT"])
                        p0 = 32 * (tt_i % 4)
                        cp("act", vc_aug[p0:p0 + 32, tt_i // 4, g, 0:64], pb[5][0:32, 64 + g * 64:128 + g * 64], [PB[5]], ["vc_aug"])
                s.emit()

            s.enabled = _LV >= 2

            with contextlib.ExitStack() as ph:
                wq_bf = TT(ph, "wq_bf", [128, 8, 1024], BF16)
                wgn_bf = TT(ph, "wgn_bf", [128, 8, 24], BF16)
                QS = [[TT(ph, "QS%d_%d" % (g, hf), [128, 4, 128], BF16) for hf in range(2)] for g in range(2)]
                ABtL = [TT(ph, "ABt%d" % i, [128, 2, 128], F32) for i in range(2)]
                vmt = TT(ph, "vmt", [128, 128], F32)
                cmtL = [TT(ph, "cmt%d" % i, [128, 4, 512], BF16) for i in range(2)]
                ropejL = [TT(ph, "ropej%d" % i, [128, 2, 512], F32) for i in range(2)]
                jk = {}
                esink = TT(ph, "esink", [128, 8], F32)
                qnu = TT(ph, "qnu", [128, 4, 128], BF16)
                qnr = TT(ph, "qnr", [128, 4, 128], BF16)
                qsr = TT(ph, "qsr", [128, 4, 128], BF16)
                tmpA = [TT(ph, "tmpA", [128, 512], F32)]
                tmpB = [TT(ph, "tmpB", [128, 512], F32)]
                gn = TT(ph, "gn", [128, 24], F32)
                imp = TT(ph, "imp", [128, 128], F32)
                imw = TT(ph, "imw", [128, 128], F32)
                m8 = TT(ph, "m8", [128, 16], F32)
                o_f = TT(ph, "o_f", [128, 1024], F32)
                o_bf = TT(ph, "o_bf", [128, 1024], BF16)

                wq_v = w_q.rearrange("(dc p) c -> p dc c", p=128)
                for i in range(4):
                    load_cast(wq_bf[:, 2 * i:2 * i + 2, :], wq_v[:, 2 * i:2 * i + 2, :], 2048,
                              lambda a: a.rearrange("p (a b) -> p a b", a=2), "wq_bf")
                load_cast(wgn_bf[:], w_gn.rearrange("(dc p) c -> p dc c", p=128), 192,
                          lambda a: a.rearrange("p (a b) -> p a b", a=8), "wgn_bf")
                xoj = [TT(ph, "xojB%d" % i, [128, 8, 128], BF16) for i in range(2)]
                dma(vmt[:], validm, [], ["vmt"])
                dma(esink[:], sinksD, [], ["esink"])
                act(esink[:, :], esink[:, :], AF.Exp, ["esink"], ["esink"])

                def rope_to(dst, ps, tab, n, rkeys, wkeys, psk):
                    tt("dve", tmpA[0][:, 0:n], ps, tab[:, 0, :], ALU.mult, [psk] + rkeys, ["tmpA"])
                    for (o0, i0) in ((0, 32), (32, 0), (64, 96), (96, 64)):
                        tt("dve", tmpB[0][o0:o0 + 32, 0:n], ps[i0:i0 + 32, :], tab[o0:o0 + 32, 1, :], ALU.mult,
                           [psk] + rkeys, ["tmpB_%d" % o0])
                    tt("dve", dst, tmpA[0][:, 0:n], tmpB[0][:, 0:n], ALU.add,
                       ["tmpA"] + ["tmpB_%d" % o for o in (0, 32, 64, 96)], wkeys)

                NEG = 30000.0
                mb4 = TT(ph, "mb4", [128, 6, 512], BF16)
                dma(mb4[:], mb4D, [], ["mb4"])
                M_CAUSAL, M_ANTI, M_W0, M_W1, M_W2, M_S2 = range(6)
                negq = [TT(ph, "negq%d" % g, [128, 128], BF16) for g in range(2)]
                Et3 = [TT(ph, "Et3_%d" % i, [128, 4, 128], BF16) for i in range(3)]
                o_cmp = TT(ph, "o_cmp", [128, 512], F32)
                o_sel = TT(ph, "o_sel", [128, 512], F32)
                o_win = TT(ph, "o_win", [128, 512], F32)
                den2 = [TT(ph, "den2_%d" % i, [128, 4], F32) for i in range(8)]
                coef2 = [TT(ph, "coef2_%d" % i, [128, 4], F32) for i in range(8)]
                pipe = {"i": 0, "pending": None}

                def flush():
                    p = pipe["pending"]
                    if p is None:
                        return
                    pipe["pending"] = None
                    (eb, vaug, vkey, kt, g, accs, first, last, post) = p
                    for hh in range(4):
                        ab_, col, ncol = accs[hh]
                        st_flag = first and (hh == 0 or accs[hh][0] != accs[hh - 1][0])
                        mm(pb[ab_][:, col:col + ncol], Et3[eb][:, hh, :], vaug[:, kt, g, :], st_flag, last and hh == 3,
                           ["Et3_%d" % eb, vkey], [PB[ab_]])
                    if last and post is not None:
                        post()

                def tile(kT, kkey, vaug, vkey, kt, g, q, qkey, accs, first, last, biases, post=None, fullk=False):
                    i = pipe["i"]
                    pipe["i"] += 1
                    sb = i % 2
                    eb = i % 3
                    nb = len(biases)
                    if fullk:
                        mm(pb[sb][:, :], kT[:, kt * 128:(kt + 1) * 128], q[:, :, :], True, nb == 0, kkey + qkey, [PB[sb]])
                    else:
                        mm(pb[sb][:, :], kT[64 * g:64 * g + 64, kt * 128:(kt + 1) * 128], q[64 * g:64 * g + 64, :, :], True, nb == 0,
                           [kkey, qkey], [PB[sb]])
                    for bi, (bl, br_, bk) in enumerate(biases):
                        mm(pb[sb][:, :], bl, br_, False, bi == nb - 1, bk, [PB[sb]])
                    flush()
                    act(Et3[eb][:, :, :], pb[sb][:, :].rearrange("p (h q) -> p h q", h=4), AF.Exp, [PB[sb]], ["Et3_%d" % eb], scale=0.125)
                    pipe["pending"] = (eb, vaug, vkey, kt, g, accs, first, last, post)

                def bias_const(mi):
                    return (ident_bf, mb4[:, mi, :], ["consts", "mb4"])

                def acc65(bank):
                    return [(bank, hh * 65, 65) for hh in range(4)]

                def finish_branch(bank, g, gate_off, dst, dkey_pfx, slot, sink=False):
                    d, c = den2[slot], coef2[slot]
                    dk, ck = "den2_%d" % slot, "coef2_%d" % slot
                    dview = pb[bank][:, 0:260].rearrange("p (h c) -> p h c", c=65)[:, :, 64:65]
                    if sink:
                        tt("dve", d[:, :].rearrange("p (h o) -> p h o", o=1), dview,
                           esink[:, 4 * g:4 * g + 4].rearrange("p (h o) -> p h o", o=1), ALU.add, [PB[bank], "esink"], [dk])
                    else:
                        cp("dve", d[:, :].rearrange("p (h o) -> p h o", o=1), dview, [PB[bank]], [dk])
                    s.op("dve", lambda e: e.reciprocal(d[:, :], d[:, :]), [dk], [dk])
                    for hh in range(4):
                        h = 4 * g + hh
                        if gate_off is None:
                            sc, sk = d[:, hh:hh + 1], dk
                        else:
                            tt("dve", c[:, hh:hh + 1], d[:, hh:hh + 1], gn[:, 3 * h + gate_off:3 * h + gate_off + 1], ALU.mult, [dk, "gn"], [ck + "_%d" % hh])
                            sc, sk = c[:, hh:hh + 1], ck + "_%d" % hh
                        ts("dve", dst[:, h * 64:(h + 1) * 64], pb[bank][:, hh * 65:hh * 65 + 64], sc, ALU.mult,
                           [PB[bank], sk], [dkey_pfx + "%d" % h])

                def finish_cmp(banks, g, slot):
                    d, c = den2[slot], coef2[slot]
                    dk, ck = "den2_%d" % slot, "coef2_%d" % slot
                    for bk in range(2):
                        cp("dve", d[:, 2 * bk:2 * bk + 2].rearrange("p (h o) -> p h o", o=1),
                           pb[banks[bk]][:, :].rearrange("p (h c) -> p h c", c=256)[:, :, 64:65], [PB[banks[bk]]], [dk])
                    ts("dve", d[:, :], d[:, :], 1e-30, ALU.max, [dk], [dk])
                    s.op("dve", lambda e: e.reciprocal(d[:, :], d[:, :]), [dk], [dk])
                    for hh in range(4):
                        bk, col = banks[hh // 2], (hh % 2) * 256
                        if hh == 0:
                            ts("dve", imp[:, :], pb[bk][:, col + 65:col + 193], d[:, hh:hh + 1], ALU.mult, [PB[bk], dk], ["imp"])
                        else:
                            stt("dve", imp[:, :], pb[bk][:, col + 65:col + 193], d[:, hh:hh + 1], imp[:, :], ALU.mult, ALU.add,
                                [PB[bk], dk, "imp"], ["imp"])
                    for hh in range(4):
                        h = 4 * g + hh
                        bk, col = banks[hh // 2], (hh % 2) * 256
                        tt("dve", c[:, hh:hh + 1], d[:, hh:hh + 1], gn[:, 3 * h:3 * h + 1], ALU.mult, [dk, "gn"], [ck + "_%d" % hh])
                        ts("dve", o_cmp[:, h * 64:(h + 1) * 64], pb[bk][:, col:col + 64], c[:, hh:hh + 1], ALU.mult,
                           [PB[bk], ck + "_%d" % hh], ["o_cmp%d" % h])
                    tt("dve", imp[:, :], imp[:, :], ABt[:, 0, :], ALU.mult, ["imp", jk["ABt"]], ["imp"])
                    tt("dve", imp[:, :], imp[:, :], ABt[:, 1, :], ALU.add, ["imp", jk["ABt"]], ["imp"])
                    s.op("dve", lambda e: e.max(out=m8[:, 0:8], in_=imp[:, :]), ["imp"], ["m8a"])
                    s.op("dve", lambda e: e.match_replace(out=imw[:, :], in_to_replace=m8[:, 0:8], in_values=imp[:, :], imm_value=-1e30),
                         ["imp", "m8a"], ["imw"])
                    s.op("dve", lambda e: e.max(out=m8[:, 8:16], in_=imw[:, :]), ["imw"], ["m8b"])
                    ts("dve", imw[:, :], imp[:, :], m8[:, 15:16], ALU.is_ge, ["imp", "m8b", "imw"], ["imw"])
                    tt("dve", imw[:, :], imw[:, :], vmt[:, :], ALU.mult, ["imw", "vmt"], ["imw"])
                    ts("dve", negq[g][:, :], imw[:, :], NEG, ALU.mult, ["imw"], ["negq%d" % g], -NEG, ALU.add)

                load_xoj(xoj[0], 0, "xoj0")
                for j in range(NJ):
                    jb = slice(j * 128, (j + 1) * 128)
                    xs = j % 2
                    xh = xoj_dma(j + 1) if j + 1 < NJ else None
                    if j == 0:
                        dma(ropejL[0][:], ropeo[:, 0, :, :], [], ["ropej0"])
                        dma(cmtL[0][:], cmaskT[:, 0, :, :], [], ["cmt0"])
                        dma(ABtL[0][:], AB[:, 0, :, :], [], ["ABt0"])
                    if j + 1 < NJ:
                        dma(ropejL[1 - xs][:], ropeo[:, j + 1, :, :], [], ["ropej%d" % (1 - xs)])
                        dma(cmtL[1 - xs][:], cmaskT[:, j + 1, :, :], [], ["cmt%d" % (1 - xs)])
                        dma(ABtL[1 - xs][:], AB[:, j + 1, :, :], [], ["ABt%d" % (1 - xs)])
                    ropej, cmt, ABt = ropejL[xs], cmtL[xs], ABtL[xs]
                    jk["ABt"], jk["cmt"], jk["ropej"] = "ABt%d" % xs, "cmt%d" % xs, "ropej%d" % xs
                    for qc in range(8):
                        b = 4 + qc // 4
                        for dc in range(8):
                            mm(pb[b][:, (qc % 4) * 128:(qc % 4 + 1) * 128], wq_bf[:, dc, qc * 128:(qc + 1) * 128], xoj[xs][:, dc, :],
                               dc == 0, dc == 7, ["wq_bf", "xoj%d" % xs], [PB[b]])
                    cp("act", qnu[:, :, :], pb[4][:, :].rearrange("p (h q) -> p h q", h=4), [PB[4]], ["qnu"])
                    rope_to(qnr[:, :, :].rearrange("p h q -> p (h q)"), pb[4][:, :], ropej, 512, [jk["ropej"]], ["qnr"], PB[4])
                    rope_to(qsr[:, :, :].rearrange("p h q -> p (h q)"), pb[5][:, :], ropej, 512, [jk["ropej"]], ["qsr"], PB[5])
                    nhalf = 1 if 4 * j + 4 <= 32 else 2
                    for g in range(2):
                        for hf in range(nhalf):
                            cp("pool", QS[g][hf][0:64, :, :], qnr[64 * g:64 * g + 64, :, :], ["qnr"], ["QSq%d_%d" % (g, hf)])
                    for dc in range(8):
                        mm(pb[2][:, 0:24], xoj[xs][:, dc, :], wgn_bf[:, dc, :], dc == 0, dc == 7, ["xoj%d" % xs, "wgn_bf"], [PB[2]])
                    act(gn[:, :], pb[2][:, 0:24], AF.Sigmoid, [PB[2]], ["gn"])
                    nct = min(4, (32 * j + 32 + 127) // 128)
                    for g in range(2):
                        banks = (4, 5) if g == 0 else (6, 7)
                        accs = [(banks[hh // 2], (hh % 2) * 256, 193) for hh in range(4)]
                        for ct in range(nct):
                            tile(kcT, "kcT", vc_aug, "vc_aug", ct, g, qnu, "qnu", accs, ct == 0, ct == nct - 1,
                                 [(ident_bf, cmt[:, ct, :], ["consts", jk["cmt"]])],
                                 post=(lambda g_=g, banks_=banks: finish_cmp(banks_, g_, g_)))
                    for g in range(2):
                        wbank, sbank = (3, 2) if g == 0 else (4, 5)
                        tiles = [t_ for t_ in range(4 * j - 1, 4 * j + 4) if t_ >= 0]
                        for ii, kt in enumerate(tiles):
                            if j == 0:
                                bl = [bias_const((M_W0, M_W1, M_W2, M_CAUSAL)[kt])]
                            elif kt == 4 * j - 1:
                                bl = [bias_const(M_ANTI)]
                            elif kt == 4 * j + 3:
                                bl = [bias_const(M_CAUSAL)]
                            else:
                                bl = []
                            tile(kwT, "kwT", vw_aug, "vw_aug", kt, g, qnr, "qnr", acc65(wbank), ii == 0, ii == len(tiles) - 1, bl,
                                 post=(lambda g_=g, b_=wbank: finish_branch(b_, g_, 2, o_win, "o_win", 2 + g_)))
                        for ii, kt in enumerate((2 * j, 2 * j + 1)):
                            bl = [bias_const(M_CAUSAL if ii == 1 else (M_S2 if j == 0 else M_ANTI))]
                            tile(kswT, "kswT", vsw_aug, "vsw_aug", kt, g, qsr, "qsr", acc65(sbank), ii == 0, ii == 1, bl,
                                 post=(lambda g_=g, b_=sbank: finish_branch(b_, g_, None, o_f[:, 512:1024], "o_s", 4 + g_, sink=True)))
                    flush()
                    if xh is not None:
                        lc_cast(xh, xoj[1 - xs][:, :, :], "xoj%d" % (1 - xs), "pool")
                    for g in range(2):
                        tr(pb[2][:, :].bitcast(BF16)[:, 0:128], negq[g][:, :], ident_bf, ["negq%d" % g, "consts"], [PB[2]])
                        for hf in range(nhalf):
                            for hh in range(4):
                                cp("act" if hh % 2 == 0 else "dve", QS[g][hf][64:128, hh, :], pb[2][:, :].bitcast(BF16)[64 * hf:64 * hf + 64, 0:128],
                                   [PB[2]], ["QSn%d_%d_%d" % (g, hf, hh)])
                    nkt = 4 * j + 4
                    for g in range(2):
                        sbank = 6 + g
                        for kt in range(nkt):
                            hf = kt // 32
                            bl = [bias_const(M_CAUSAL)] if kt == nkt - 1 else []
                            qk = ["QSq%d_%d" % (g, hf)] + ["QSn%d_%d_%d" % (g, hf, hh) for hh in range(4)]
                            tile(KI[g], ["KI", "KIind%d" % g], vs_aug, "vs_aug", kt, g, QS[g][hf], qk, acc65(sbank), kt == 0, kt == nkt - 1, bl,
                                 post=(lambda g_=g, b_=sbank: finish_branch(b_, g_, 1, o_sel, "o_sel", 6 + g_)), fullk=True)
                    flush()
                    tt("dve", o_f[:, 0:512], o_cmp[:, :], o_sel[:, :], ALU.add,
                       ["o_cmp%d" % h for h in range(8)] + ["o_sel%d" % h for h in range(8)], ["o_fa"])
                    tt("dve", o_f[:, 0:512], o_f[:, 0:512], o_win[:, :], ALU.add, ["o_fa"] + ["o_win%d" % h for h in range(8)], ["o_fb"])
                    cp("dve", o_bf[:, :], o_f[:, :], ["o_fb"] + ["o_s%d" % h for h in range(8)], ["o_bf"])
                    dma(obuf[jb, :], o_bf[:, :], ["o_bf"], ["obuf%d" % j])
                s.emit()
        s.enabled = _LV >= 3
        mid = top.enter_context(contextlib.ExitStack())
        idx_all = TT(mid, "idx_all", [128, NJ, 4], I32)
        gates_all = TT(mid, "gates_all", [128, NJ, 4], F32)
        GT_all = TT(mid, "GT_all", [32, NJ, 128], F32)

        def layer_norm(src, dst, lnt, stats, mv, rstd, tmp, skey, dkey, pfx):
            for hf in range(2):
                s.op("dve", (lambda h_: (lambda e: e.bn_stats(stats[:, h_, :], src[:, h_ * 512:(h_ + 1) * 512])))(hf),
                     [skey], [pfx + "st%d" % hf])
            s.op("dve", lambda e: e.bn_aggr(mv[:, :], stats[:, :, :]), [pfx + "st0", pfx + "st1"], [pfx + "mv"])
            ts("dve", rstd[:, :], mv[:, 1:2], EPS, ALU.add, [pfx + "mv"], [pfx + "rs"])
            s.op("act", lambda e: e.sqrt(rstd[:, :], rstd[:, :]), [pfx + "rs"], [pfx + "rs"])
            s.op("dve", lambda e: e.reciprocal(rstd[:, :], rstd[:, :]), [pfx + "rs"], [pfx + "rs"])
            ts("dve", tmp[:, :], src[:, :], mv[:, 0:1], ALU.subtract, [skey, pfx + "mv", pfx + "rs"], [pfx + "tmp"], rstd[:, 0:1], ALU.mult)
            tt("dve", tmp[:, :], tmp[:, :], lnt[:, 0, :], ALU.mult, [pfx + "tmp", pfx + "lnt"], [pfx + "tmp"])
            tt("dve", dst, tmp[:, :], lnt[:, 1, :], ALU.add, [pfx + "tmp", pfx + "lnt"], [dkey])

        with contextlib.ExitStack() as ph:
            wbr_bf = TT(ph, "wbr_bf", [128, 2, 4, DM], BF16)
            wout_bf = TT(ph, "wout_bf", [128, 8, DM], BF16)
            wr_sb = TT(ph, "wr_sb", [128, 8, 32], F32)
            br_sb = TT(ph, "br_sb", [128, 32], F32)
            ecap = TT(ph, "ecap", [128, 32], F32)
            base_bc = TT(ph, "base_bc", [128, 32], F32)
            o_bf = TT(ph, "o_bf2", [128, 1024], BF16)
            lnt1 = TT(ph, "lnt1", [128, 2, DM], F32)
            dma(lnt1[:], lnD[:, 0:2, :], [], ["l1lnt"])
            oT = TT(ph, "oT", [128, 8, 128], BF16)
            sgm = TT(ph, "sgm", [128, 2048], BF16)
            t1 = TT(ph, "t1", [128, 1024], F32)
            t2 = TT(ph, "t2", [128, 1024], F32)
            mix_bf = TT(ph, "mix_bf", [128, 1024], BF16)
            mixT = TT(ph, "mixT", [128, 8, 128], BF16)
            xot = TT(ph, "xot", [128, DM], F32)
            u = TT(ph, "u", [128, DM], F32)
            hs = TT(ph, "hs", [128, DM], F32)
            h_bf = TT(ph, "h_bf", [128, DM], BF16)
            hT = TT(ph, "hT", [128, 8, 128], F32)
            stats = TT(ph, "stats", [128, 2, 6], F32)
            mv = TT(ph, "mv", [128, 2], F32)
            rstd = TT(ph, "rstd", [128, 1], F32)
            lg = TT(ph, "lg", [128, 32], F32)
            r8 = TT(ph, "r8", [128, 8], F32)
            Mf = TT(ph, "Mf", [128, 32], F32)
            Mb = TT(ph, "Mb", [128, 32], BF16)
            ex = TT(ph, "ex", [128, 32], F32)
            Gt = TT(ph, "Gt", [128, 32], F32)
            sm1 = TT(ph, "sm1", [128, 1], F32)
            rank = TT(ph, "rank", [128, 32], F32)
            vld = TT(ph, "vld", [128, 32], F32)
            slotm = TT(ph, "slotm", [128, 32], F32)
            s8 = TT(ph, "s8", [128, 8], F32)
            fix = TT(ph, "fix", [128, 4], F32)
            oh = TT(ph, "oh", [128, 32], F32)
            for m_ in range(2):
                wb_v = wbr[m_].rearrange("(ch p) c -> p ch c", p=128)
                for i in range(2):
                    load_cast(wbr_bf[:, m_, 2 * i:2 * i + 2, :], wb_v[:, 2 * i:2 * i + 2, :], 2048,
                              lambda a: a.rearrange("p (a b) -> p a b", a=2), "wbr_bf")
            wo_v = wout.rearrange("(ch p) c -> p ch c", p=128)
            for i in range(4):
                load_cast(wout_bf[:, 2 * i:2 * i + 2, :], wo_v[:, 2 * i:2 * i + 2, :], 2048,
                          lambda a: a.rearrange("p (a b) -> p a b", a=2), "wout_bf")
            dma(wr_sb[:], wrD.rearrange("(dc p) e -> p dc e", p=128), [], ["wr_sb"])
            dma(br_sb[:], brD, [], ["br_sb"])
            dma(ecap[:], ecapD, [], ["ecap"])
            s.op("dve", lambda e: e.memset(base_bc[:, :], 0.0), [], ["base_bc"])
            sgmL = [sgm, TT(ph, "sgm_b", [128, 2048], BF16)]
            wgm2 = TT(ph, "wgm2", [128, 8, 2048], BF16)
            wgm_v2 = w_gm.rearrange("(dc p) c -> p dc c", p=128)
            for i in range(8):
                load_cast(wgm2[:, i, :], wgm_v2[:, i, :], 2048, lambda a: a, "wgm2")
            xojC = [TT(ph, "xojC%d" % i, [128, 8, 128], BF16) for i in range(2)]
            load_xoj(xojC[0], 0, "xojC0")
            xotL = [xot, TT(ph, "xot_b", [128, DM], F32)]
            obfL = [o_bf, TT(ph, "o_bf2b", [128, 1024], BF16)]
            uL = [u, TT(ph, "u_b", [128, DM], F32)]
            lntmp = TT(ph, "lntmp", [128, DM], F32)

            def stage1(j):
                jb = slice(j * 128, (j + 1) * 128)
                sl = j % 2
                sgm, xot, o_bf, u = sgmL[sl], xotL[sl], obfL[sl], uL[sl]
                sgk, xok, obk, uk = "sgm%d" % sl, "xot%d" % sl, "o_bf%d" % sl, "u%d" % sl
                xh_ = xoj_dma(j + 1) if j + 1 < NJ else None
                for cc in range(4):
                    gb = (3, 7)[cc % 2]
                    for dc in range(8):
                        mm(pb[gb][:, :], xojC[sl][:, dc, :], wgm2[:, dc, cc * 512:(cc + 1) * 512], dc == 0, dc == 7,
                           ["xojC%d" % sl, "wgm2"], [PB[gb]])
                    act(sgm[:, cc * 512:(cc + 1) * 512], pb[gb][:, :], AF.Sigmoid, [PB[gb]], [sgk + "_%d" % cc])
                dma(xot[:], xo[jb, :], [], [xok])
                dma(o_bf[:, :], obuf[jb, :], ["obuf%d" % j], [obk])
                for ch in range(8):
                    tr(pb[6][:, :].bitcast(BF16)[:, ch * 128:(ch + 1) * 128], o_bf[:, ch * 128:(ch + 1) * 128], ident_bf,
                       [obk, "consts"], [PB[6]])
                cp("act", oT[:, :, :], pb[6][:, :].bitcast(BF16).rearrange("p (c q) -> p c q", c=8), [PB[6]], ["oT"])
                for m_ in range(2):
                    for hf in range(2):
                        b = 4 + hf if m_ == 0 else 6 + hf
                        for ch in range(4):
                            mm(pb[b][:, :], oT[:, 4 * m_ + ch, :], wbr_bf[:, m_, ch, hf * 512:(hf + 1) * 512], ch == 0, ch == 3,
                               ["oT", "wbr_bf"], [PB[b]])
                for hf in range(2):
                    hs_ = slice(hf * 512, (hf + 1) * 512)
                    tt("dve", t1[:, hs_], pb[4 + hf][:, :], sgm[:, hf * 512:(hf + 1) * 512], ALU.mult, [PB[4 + hf], sgk + "_%d" % hf], ["t1_%d" % hf])
                    tt("dve", t2[:, hs_], pb[6 + hf][:, :], sgm[:, 1024 + hf * 512:1024 + (hf + 1) * 512], ALU.mult, [PB[6 + hf], sgk + "_%d" % (2 + hf)], ["t2_%d" % hf])
                tt("dve", mix_bf[:, :], t1[:, :], t2[:, :], ALU.add, ["t1_0", "t1_1", "t2_0", "t2_1"], ["mix_bf"])
                for ch in range(8):
                    tr(pb[6][:, :].bitcast(BF16)[:, ch * 128:(ch + 1) * 128], mix_bf[:, ch * 128:(ch + 1) * 128], ident_bf,
                       ["mix_bf", "consts"], [PB[6]])
                cp("act", mixT[:, :, :], pb[6][:, :].bitcast(BF16).rearrange("p (c q) -> p c q", c=8), [PB[6]], ["mixT"])
                for hf in range(2):
                    for ch in range(8):
                        mm(pb[4 + hf][:, :], mixT[:, ch, :], wout_bf[:, ch, hf * 512:(hf + 1) * 512], ch == 0, ch == 7,
                           ["mixT", "wout_bf"], [PB[4 + hf]])
                for hf in range(2):
                    hs_ = slice(hf * 512, (hf + 1) * 512)
                    stt("dve", u[:, hs_], xot[:, hs_], ALPHA, pb[4 + hf][:, :], ALU.mult, ALU.add, [xok, PB[4 + hf]], [uk + "_%d" % hf])
                s.op("dve", lambda e: e.engine_nop(), [uk + "_0", uk + "_1"], [uk])
                if xh_ is not None:
                    lc_cast(xh_, xojC[1 - sl][:, :, :], "xojC%d" % (1 - sl), "act")

            def stage2(j):
                jb = slice(j * 128, (j + 1) * 128)
                sl = j % 2
                u = uL[sl]
                uk = "u%d" % sl
                layer_norm(u, hs[:, :], lnt1, stats, mv, rstd, lntmp, uk, "hs", "l1")
                dma(hbuf[jb, :], hs[:, :], ["hs"], ["hbuf%d" % j])
                cp("act", h_bf[:, :], hs[:, :], ["hs"], ["h_bf"])
                for dc in range(8):
                    b = dc // 4
                    tr(pb[b][:, (dc % 4) * 128:(dc % 4 + 1) * 128], hs[:, dc * 128:(dc + 1) * 128], identf[:, :], ["hs", "identf"], [PB[b]])
                for bk in range(2):
                    cp("act" if bk == 0 else "dve", hT[:, 4 * bk:4 * bk + 4, :], pb[bk][:, :].rearrange("p (c q) -> p c q", c=4), [PB[bk]], ["hT%d" % bk])
                for dc in range(8):
                    mm(pb[2][:, 0:32], hT[:, dc, :], wr_sb[:, dc, :], dc == 0, dc == 7, ["hT0", "hT1", "wr_sb"], [PB[2]])
                tt("dve", lg[:, :], pb[2][:, 0:32], br_sb[:, :], ALU.add, [PB[2], "br_sb"], ["lg"])
                s.op("dve", lambda e: e.max(out=r8[:, :], in_=lg[:, :]), ["lg"], ["r8"])
                ts("dve", Mf[:, :], lg[:, :], r8[:, 3:4], ALU.is_ge, ["lg", "r8"], ["Mf"])
                cp("dve", Mb[:, :], Mf[:, :], ["Mf"], ["Mb"])
                ts("dve", sm1[:, :], r8[:, 0:1], -1.0, ALU.mult, ["r8"], ["sm1"])
                act(ex[:, :], lg[:, :], AF.Exp, ["lg", "sm1"], ["ex"], bias=sm1[:, 0:1])
                tt("dve", ex[:, :], ex[:, :], Mf[:, :], ALU.mult, ["ex", "Mf"], ["ex"])
                s.op("dve", lambda e: e.reduce_sum(sm1[:, :], ex[:, :], AX.X), ["ex", "sm1"], ["sm1"])
                s.op("dve", lambda e: e.reciprocal(sm1[:, :], sm1[:, :]), ["sm1"], ["sm1"])
                ts("dve", Gt[:, :], ex[:, :], sm1[:, 0:1], ALU.mult, ["ex", "sm1"], ["Gt"])
                mm(pb[2][:, 32:64], consts[:, C_TRIU, :], Mb[:, :], True, True, ["consts", "Mb"], [PB[2]])
                mm(pb[2][:, 64:96], consts[:, C_ONES, :], Mb[:, :], True, True, ["consts", "Mb"], [PB[2]])
                tt("dve", rank[:, :], pb[2][:, 32:64], base_bc[:, :], ALU.add, [PB[2], "base_bc"], ["rank"])
                tt("dve", base_bc[:, :], pb[2][:, 64:96], base_bc[:, :], ALU.add, [PB[2], "base_bc"], ["base_bc"])
                ts("dve", vld[:, :], rank[:, :], float(CAP), ALU.is_lt, ["rank"], ["vld"])
                tt("dve", vld[:, :], vld[:, :], Mf[:, :], ALU.mult, ["vld", "Mf"], ["vld"])
                tt("dve", slotm[:, :], rank[:, :], ecap[:, :], ALU.add, ["rank", "ecap"], ["slotm"])
                ts("dve", slotm[:, :], slotm[:, :], 1.0, ALU.add, ["slotm"], ["slotm"])
                tt("dve", slotm[:, :], slotm[:, :], vld[:, :], ALU.mult, ["slotm", "vld"], ["slotm"])
                ts("dve", slotm[:, :], slotm[:, :], -1.0, ALU.add, ["slotm"], ["slotm"])
                s.op("dve", lambda e: e.max(out=s8[:, :], in_=slotm[:, :]), ["slotm"], ["s8"])
                ts("dve", fix[:, :], s8[:, 0:4], 0.0, ALU.is_lt, ["s8"], ["fix"], float(NSLOT + 1), ALU.mult)
                tt("dve", fix[:, :], fix[:, :], s8[:, 0:4], ALU.add, ["fix", "s8"], ["fix"])
                ts("dve", fix[:, :], fix[:, :], 0.0, ALU.max, ["fix"], ["fix"], float(NSLOT), ALU.min)
                cp("dve", idx_all[:, j, :], fix[:, :], ["fix"], ["idx%d" % j])
                for k in range(4):
                    ts("dve", oh[:, :], slotm[:, :], s8[:, k:k + 1], ALU.is_equal, ["slotm", "s8"], ["oh"])
                    tt("dve", oh[:, :], oh[:, :], Gt[:, :], ALU.mult, ["oh", "Gt"], ["oh"])
                    s.op("dve", (lambda k_, j_: (lambda e: e.reduce_sum(gates_all[:, j_, k_:k_ + 1], oh[:, :], AX.X)))(k, j),
                         ["oh"], ["gates%d_%d" % (j, k)])
                    s.dma((lambda k_, j_: (lambda e: e.indirect_dma_start(
                        out=Xbuf, out_offset=bass.IndirectOffsetOnAxis(ap=idx_all[:, j_, k_:k_ + 1], axis=0),
                        in_=h_bf[:, :], in_offset=None)))(k, j),
                        ["idx%d" % j, "h_bf"], ["Xbuf_w%d_%d" % (j, k)], q="pool")
                tr(pb[2][0:32, 384:512], Gt[:, :], identf[:, :], ["Gt", "identf"], [PB[2]])
                cp("act", GT_all[:, j, :], pb[2][0:32, 384:512], [PB[2]], ["GT%d" % j])

            stage1(0)
            for j in range(NJ):
                if j + 1 < NJ:
                    stage1(j + 1)
                stage2(j)
            s.emit()

        s.enabled = _LV >= 4
        xkeys = ["Xbuf_w%d_%d" % (j, k) for j in range(NJ) for k in range(4)]
        with contextlib.ExitStack() as ph:
            win_bf = [TT(ph, "win_bf%d" % i, [128, 8, 2048], BF16) for i in range(2)]
            wo_bf = [TT(ph, "wo_bf%d" % i, [128, 8, DM], BF16) for i in range(2)]
            bei = TT(ph, "bei", [128, 32, 16], F32)
            xrow = [TT(ph, "xrow%d" % i, [128, 3, DM], BF16) for i in range(2)]
            xeT = [TT(ph, "xeT%d" % i, [128, 8, CAP], BF16) for i in range(2)]
            AT = [TT(ph, "AT%d" % i, [128, 8, CAP], BF16) for i in range(2)]
            g1 = [TT(ph, "g1_%d" % i, [128, CAP], F32) for i in range(2)]
            sgt = [TT(ph, "sgt%d" % i, [128, CAP], F32) for i in range(2)]
            u1 = [TT(ph, "u1_%d" % i, [128, CAP], F32) for i in range(2)]
            ysb = [TT(ph, "ysb%d" % i, [128, DM], F32) for i in range(2)]
            dma(bei[:], beiD, [], ["bei"])
            for i in range(3, 5):
                stg.append(TT(ph, "stg%d" % i, [128, 2048], F32))
            nstg["n"] = 5
            NSTG = 5

            def expert_tasks(e):
                sl = e % 2
                wv = wei[e].rearrange("(dc p) f -> p dc f", p=128)
                ov = weo[e].rearrange("(fc p) c -> p fc c", p=128)
                cengs = ["act", "dve", "act", "dve", "act", "pool", "act", "dve", "act", "dve", "dve", "act"]
                tasks = []
                for dc in range(8):
                    tasks.append((win_bf[sl][:, dc, :], wv[:, dc, :], "win_bf%d_%d" % (sl, dc)))
                for i in range(4):
                    tasks.append((wo_bf[sl][:, 2 * i:2 * i + 2, :], ov[:, 2 * i:2 * i + 2, :], "wo_bf%d" % sl))
                return [(d, sr, k, cengs[i]) for i, (d, sr, k) in enumerate(tasks)]

            def task_dma(t):
                i = st_state["i"] % nstg["n"]
                st_state["i"] += 1
                sv = stg[i][:, 0:2048]
                if len(t[1].shape) == 3:
                    sv = sv.rearrange("p (a b) -> p a b", a=2)
                dma(sv, t[1], [], ["stg%d" % i])
                return (sv, i)

            def task_cast(t, h):
                cp(t[3], t[0], h[0], ["stg%d" % h[1]], [t[2]])

            def xrow_dma(e):
                sl = e % 2
                dma(xrow[sl][:, :, :], Xbuf[e * CAP:(e + 1) * CAP, :].rearrange("(st p) d -> p st d", p=128), xkeys, ["xrow%d" % sl])

            t0s = expert_tasks(0)
            xrow_dma(0)
            hs0 = {}
            for c in range(len(t0s)):
                if c < NSTG:
                    hs0[c] = task_dma(t0s[c])
            for c in range(len(t0s)):
                task_cast(t0s[c], hs0[c])
                if c + NSTG < len(t0s):
                    hs0[c + NSTG] = task_dma(t0s[c + NSTG])
            casts_per_ft = [2, 1, 2, 1, 2, 1, 2, 1]
            for e in range(32):
                sl = e % 2
                nxt = expert_tasks(e + 1) if e + 1 < 32 else []
                nh = {}
                nstate = {"c": 0}
                if nxt:
                    xrow_dma(e + 1)
                    for c in range(NSTG):
                        nh[c] = task_dma(nxt[c])

                def advance(n):
                    for _ in range(n):
                        c = nstate["c"]
                        if c >= len(nxt):
                            return
                        task_cast(nxt[c], nh[c])
                        if c + NSTG < len(nxt):
                            nh[c + NSTG] = task_dma(nxt[c + NSTG])
                        nstate["c"] += 1
                for st_ in range(3):
                    for dc in range(8):
                        tr(pb[6][:, :].bitcast(BF16)[:, dc * 128:(dc + 1) * 128], xrow[sl][:, st_, dc * 128:(dc + 1) * 128], ident_bf,
                           ["xrow%d" % sl, "consts"], [PB[6]])
                    cp("act" if st_ != 1 else "dve", xeT[sl][:, :, st_ * 128:(st_ + 1) * 128],
                       pb[6][:, :].bitcast(BF16).rearrange("p (c q) -> p c q", c=8), [PB[6]], ["xeT%d_%d" % (sl, st_)])
                xek = ["xeT%d_%d" % (sl, i) for i in range(3)]
                for ft in range(8):
                    bg, bu, w2 = ft % 2, 2 + ft % 2, ft % 2
                    for dc in range(8):
                        mm(pb[bg][:, 0:CAP], win_bf[sl][:, dc, ft * 128:(ft + 1) * 128], xeT[sl][:, dc, :], dc == 0, dc == 7,
                           ["win_bf%d_%d" % (sl, d_) for d_ in range(8)] + xek, [PB[bg]])
                    for dc in range(8):
                        mm(pb[bu][:, 0:CAP], win_bf[sl][:, dc, 1024 + ft * 128:1024 + (ft + 1) * 128], xeT[sl][:, dc, :], dc == 0, dc == 7,
                           ["win_bf%d_%d" % (sl, d_) for d_ in range(8)] + xek, [PB[bu]])
                    ts("dve", g1[w2][:, :], pb[bg][:, 0:CAP], bei[:, e, ft:ft + 1], ALU.add, [PB[bg], "bei"], ["g1_%d" % w2], 7.0, ALU.min)
                    act(sgt[w2][:, :], g1[w2][:, :], AF.Silu, ["g1_%d" % w2], ["sgt%d" % w2], scale=1.702)
                    ts("dve", u1[w2][:, :], pb[bu][:, 0:CAP], bei[:, e, 8 + ft:9 + ft], ALU.add, [PB[bu], "bei"], ["u1_%d" % w2], 7.0, ALU.min)
                    ts("dve", u1[w2][:, :], u1[w2][:, :], -7.0, ALU.max, ["u1_%d" % w2], ["u1_%d" % w2], 1.0, ALU.add)
                    stt("dve", AT[sl][:, ft, :], sgt[w2][:, :], 1.0 / 1.702, u1[w2][:, :], ALU.mult, ALU.mult,
                        ["sgt%d" % w2, "u1_%d" % w2], ["AT%d_%d" % (sl, ft)])
                    advance(casts_per_ft[ft])
                atk = ["AT%d_%d" % (sl, ft) for ft in range(8)]
                for st_ in range(3):
                    ys = (e * 3 + st_) % 2
                    for hf in range(2):
                        b = 4 + hf
                        for ft in range(8):
                            mm(pb[b][:, :], AT[sl][:, ft, st_ * 128:(st_ + 1) * 128], wo_bf[sl][:, ft, hf * 512:(hf + 1) * 512], ft == 0, ft == 7,
                               atk + ["wo_bf%d" % sl], [PB[b]])
                        cp("act", ysb[ys][:, hf * 512:(hf + 1) * 512], pb[b][:, :], [PB[b]], ["ysb%d_%d" % (ys, hf)])
                    dma(Ybuf[e * CAP + st_ * 128:e * CAP + (st_ + 1) * 128, :], ysb[ys][:, :], ["ysb%d_0" % ys, "ysb%d_1" % ys], ["Ybuf_%d_%d" % (e, st_)], q="act")
                advance(99)
            s.emit()

        nstg["n"] = 3
        s.enabled = _LV >= 5
        ykeys = ["Ybuf_%d_%d" % (e, st_) for e in range(32) for st_ in range(3)]
        with contextlib.ExitStack() as ph:
            yg = [[TT(ph, "yg%d_%d" % (i, k), [128, DM], F32) for k in range(4)] for i in range(2)]
            hh_ = [TT(ph, "hh%d" % i, [128, DM], F32) for i in range(2)]
            acc = [TT(ph, "acc%d" % i, [128, DM], F32) for i in range(2)]
            tmp = TT(ph, "tmpd", [128, DM], F32)
            ot = [TT(ph, "ot%d" % i, [128, DM], F32) for i in range(2)]
            stats = TT(ph, "stats2", [128, 2, 6], F32)
            mv = TT(ph, "mv2", [128, 2], F32)
            rstd = TT(ph, "rstd2", [128, 1], F32)
            beo_sb = TT(ph, "beo_sb", [32, DM], F32)
            lnt2 = TT(ph, "lnt2", [128, 2, DM], F32)
            dma(lnt2[:], lnD[:, 2:4, :], [], ["l2lnt"])
            dma(beo_sb[:, :], beoD, [], ["beo_sb"])
            for j in range(NJ):
                sl = j % 2
                jb = slice(j * 128, (j + 1) * 128)
                dma(hh_[sl][:, :], hbuf[jb, :], ["hbuf%d" % j], ["hh%d" % sl])
                for k in range(4):
                    s.dma((lambda k_, j_, sl_: (lambda e: e.indirect_dma_start(
                        out=yg[sl_][k_][:, :], out_offset=None, in_=Ybuf,
                        in_offset=bass.IndirectOffsetOnAxis(ap=idx_all[:, j_, k_:k_ + 1], axis=0))))(k, j, sl),
                        ykeys + ["idx%d" % j], ["yg%d_%d" % (sl, k)], q="pool")
                for hf in range(2):
                    mm(pb[hf][:, :], GT_all[:, j, :], beo_sb[:, hf * 512:(hf + 1) * 512], True, True, ["GT%d" % j, "beo_sb"], [PB[hf]])
                for hf in range(2):
                    hs_ = slice(hf * 512, (hf + 1) * 512)
                    stt("dve", acc[sl][:, hs_], hh_[sl][:, hs_], ALPHA, pb[hf][:, :], ALU.mult, ALU.add, ["hh%d" % sl, PB[hf]], ["acc%d_%d" % (sl, hf)])
                akeys = ["acc%d_0" % sl, "acc%d_1" % sl]
                for k in range(4):
                    eng = "dve"
                    stt(eng, acc[sl][:, :], yg[sl][k][:, :], gates_all[:, j, k:k + 1], acc[sl][:, :], ALU.mult, ALU.add,
                        ["yg%d_%d" % (sl, k), "gates%d_%d" % (j, k)] + akeys, akeys)
                s.op("dve", lambda e: e.engine_nop(), akeys, ["acc%d" % sl])
                layer_norm(acc[sl], ot[sl][:, :], lnt2, stats, mv, rstd, tmp, "acc%d" % sl, "ot%d" % sl, "l2")
                tok = dma(yout[jb, :], ot[sl][:, :], ["ot%d" % sl], ["yout%d" % j])
                if tok is not None:
                    s.out_toks.append(tok)
            s.emit(final=True)
    return nc


_BF = ml_dtypes.bfloat16


def _common_consts():
    key = np.arange(128)[:, None]
    q = np.arange(128)[None, :]
    causal = (key <= q).astype(np.float32)
    anti = (key > q).astype(np.float32)
    ones = np.ones((128, 128), np.float32)
    triu = (key < q).astype(np.float32)
    ident = np.eye(128, dtype=np.float32)
    cs = np.arange(512) - 1
    cstart = cs * 16
    sstart = np.arange(128) * 64
    ov = ((cstart[:, None] < sstart[None, :] + 64) & (cstart[:, None] + 32 > sstart[None, :]) & (cs[:, None] >= 0)).astype(np.float32)
    ovl = ov.reshape(4, 128, 128).transpose(1, 0, 2)
    Ex = (np.arange(S)[None, :] // 64 == np.arange(128)[:, None]).astype(np.float32)
    return causal, anti, ones, triu, ident, ovl, Ex


def _rope_tab(pos):
    half = 32
    inv = (np.float32(10000.0) ** (-np.arange(half, dtype=np.float32) / np.float32(half))).astype(np.float32)
    ang = pos.astype(np.float32)[:, None] * inv[None, :]
    cos = np.cos(ang).astype(np.float32).T
    sin = np.sin(ang).astype(np.float32).T
    p = np.arange(128)
    c = cos[p % 32]
    sgn = np.where((p % 64) < 32, -1.0, 1.0).astype(np.float32)[:, None]
    sS = sin[p % 32] * sgn
    return np.stack([c, sS], 1).astype(np.float32)


def kernel(x, w_in, nsa_k_pe, nsa_k_w1, nsa_k_w2, nsa_v_pe, nsa_v_w1, nsa_v_w2, swa_sinks,
           w_br_nsa, w_br_swa, w_out, ln1_g, ln1_b, w_router, b_router, w_expert_in, b_expert_in,
           w_expert_out, b_expert_out, ln2_g, ln2_b):
    f = lambda a: np.ascontiguousarray(np.asarray(a, dtype=np.float32))
    x = f(x); w = f(w_in)[0]
    causal, anti, ones, triu, ident, ovl, Ex = _common_consts()
    qn, kc, vc, ks, vs, kw, vw, gn, qs, ksw, vsw, gm = np.split(w, np.cumsum(
        [512, 128, 128, 128, 128, 128, 128, 24, 512, 128, 128])[:], axis=1)

    def qperm(m):
        hs = m.reshape(DM, 8, 64)
        return np.concatenate([np.concatenate([hs[:, hh], hs[:, 4 + hh]], 1) for hh in range(4)], 1)

    shared = {
        "w_kvf": f(np.concatenate([kc, vc, ks, kw, ksw], 1)),
        "w_vt": f(np.concatenate([vs, vw, vsw], 1)),
        "w_q": f(np.concatenate([qperm(qn), qperm(qs)], 1)),
        "w_gn": f(gn), "w_gm": f(gm),
        "cw1": f(np.stack([f(nsa_k_w1)[0], f(nsa_v_w1)[0]], 0)),
        "cw2": f(np.stack([f(nsa_k_w2)[0], f(nsa_v_w2)[0]], 1)),
        "ovl": ovl.astype(_BF), "ind": ((np.arange(S)[None, :] // 64) % 64 == np.arange(64)[:, None]).astype(np.float32).astype(_BF), "identf": ident,
        "sinks": f(np.broadcast_to(f(swa_sinks)[0][None, :], (128, 8))),
        "wbr": f(np.stack([f(w_br_nsa)[0], f(w_br_swa)[0]], 0)), "wout": f(w_out)[0],
        "ln": f(np.broadcast_to(np.stack([f(ln1_g)[0], f(ln1_b)[0], f(ln2_g)[0], f(ln2_b)[0]], 0)[None], (128, 4, DM))),
        "wr": f(w_router)[0], "br": f(np.broadcast_to(f(b_router)[0][None, :], (128, 32))),
        "ecap": f(np.broadcast_to((np.arange(32, dtype=np.float32) * CAP)[None, :], (128, 32))),
        "wei": f(w_expert_in)[0], "weo": f(w_expert_out)[0], "beo": f(b_expert_out)[0],
        "bei": f(f(b_expert_in)[0].reshape(32, 16, 128).transpose(2, 0, 1)),
    }
    pes = []
    for pe in (f(nsa_k_pe)[0], f(nsa_v_pe)[0]):
        pes.append(pe.reshape(16, 2, 64).transpose(1, 2, 0).reshape(128, 16))
    shared["pe2"] = f(np.stack(pes, 1))

    in_maps = []
    for c in range(8):
        b, r = c // 4, c % 4
        pad = (3 - r) * 128
        xT = x[b].T
        xTf = np.zeros((DM, S), np.float32)
        xTf[:, pad:] = xT[:, :S - pad]
        own = np.concatenate([np.arange((4 * j + r) * 128, (4 * j + r + 1) * 128) for j in range(NJ)])
        m = dict(shared)
        m["xTf"] = xTf
        m["xTo"] = f(xT[:, own])
        m["xo"] = f(x[b][own])
        m["ropef"] = _rope_tab(np.maximum(np.arange(S) - pad, 0))
        ro = _rope_tab(own)
        ro = ro.reshape(128, 2, NJ, 1, 128).transpose(0, 2, 1, 3, 4)
        m["ropeo"] = f(np.broadcast_to(ro, (128, NJ, 2, 4, 128)).reshape(128, NJ, 2, 512))
        cidx = np.arange(512)
        cs_real_start = 16 * (cidx - 1) - pad
        cvalid = (cidx >= 1) & (cs_real_start >= 0)
        cm = cvalid[:, None] & ((cs_real_start + 31)[:, None] <= own[None, :])
        cm = cm.reshape(4, 128, NJ, 1, 128).transpose(1, 2, 0, 3, 4)
        cmb = np.where(np.broadcast_to(cm, (128, NJ, 4, 4, 128)), 0.0, -30000.0).astype(np.float32)
        m["cmaskT"] = np.ascontiguousarray(cmb.reshape(128, NJ, 4, 512)).astype(_BF)
        jr = np.arange(128) - 2 * (3 - r)
        cur = own // 64
        jrr = jr[None, :]
        fut = jrr > cur[:, None]
        padb = jrr < 0
        forced = (jrr == 0) | (jrr == cur[:, None]) | (jrr == cur[:, None] - 1)
        A = np.where(fut | padb | forced, 0.0, 1.0).astype(np.float32)
        Bm = np.where(fut | padb, -1.0, np.where(forced, 1e6, 0.0)).astype(np.float32)
        ABm = np.stack([A, Bm], 1).reshape(NJ, 128, 2, 128).transpose(1, 0, 2, 3)
        m["AB"] = f(ABm)
        m["validm"] = f(np.broadcast_to((jr >= 0).astype(np.float32)[None, :], (128, 128)))
        w0 = ones * float(0 >= 3 - r); w1 = ones * float(1 >= 3 - r); w2 = ones * float(2 >= 3 - r)
        s2 = anti * float(r >= 1)
        m["consts"] = np.stack([causal, anti, w0, w1, w2, s2, triu, ones, ident], 1).astype(_BF)
        mb = [np.where(np.tile(mk_, (1, 4)) > 0.5, 0.0, -30000.0).astype(np.float32) for mk_ in (causal, anti, w0, w1, w2, s2)]
        m["mb4"] = np.stack(mb, 1).astype(_BF)
        in_maps.append(m)

    nc = build_nc()
    res = run_bass_kernel_spmd(nc, in_maps, core_ids=list(range(8)))
    out = np.zeros((2, S, DM), np.float32)
    for c in range(8):
        b, r = c // 4, c % 4
        y = np.asarray(res.results[c]["y"], dtype=np.float32)
        for j in range(NJ):
            out[b, (4 * j + r) * 128:(4 * j + r + 1) * 128] = y[j * 128:(j + 1) * 128]
    return out
```

```python
import contextlib
import numpy as np
import ml_dtypes
import concourse.bass as bass
import concourse.mybir as mybir
from concourse.bass_utils import run_bass_kernel_spmd

F32 = mybir.dt.float32
BF16 = mybir.dt.bfloat16
I32 = mybir.dt.int32
AF = mybir.ActivationFunctionType
ALU = mybir.AluOpType
AX = mybir.AxisListType

S = 8192
DM = 1024
NJ = 16
CAP = 384
_LV = 9
NSLOT = 32 * CAP
ALPHA = 2.0 ** 0.25
EPS = 1e-5


class Sched:
    ENGS = ("pe", "act", "dve", "pool", "sp")
    NDMA = 24

    def __init__(self, nc, sems):
        self.nc = nc
        self.sems = sems
        self.ops = {e: [] for e in self.ENGS}
        self.seq = {e: 0 for e in self.ENGS}
        self.waited = {e: {} for e in self.ENGS}
        self.last_w = {}
        self.readers = {}
        self.dma_i = 0
        self.dma_tok = [None] * self.NDMA
        self.dma_val = [0] * self.NDMA
        self.out_toks = []
        self.enabled = True

    def _deps(self, reads, writes, eng=None):
        deps = []
        for r in reads:
            t = self.last_w.get(r)
            if t is not None:
                deps.append(t)
            if r.startswith("pb"):
                deps.extend(x for x in self.readers.get(r, ()) if x[0] != eng)
        for w in writes:
            t = self.last_w.get(w)
            if t is not None:
                deps.append(t)
            deps.extend(self.readers.get(w, ()))
        return deps

    def _commit(self, tok, reads, writes):
        for r in reads:
            self.readers.setdefault(r, []).append(tok)
        for w in writes:
            self.last_w[w] = tok
            self.readers[w] = []

    def _waits(self, eng, deps):
        need = {}
        for (k, v) in deps:
            if k == eng and eng == "pe":
                continue
            if self.waited[eng].get(k, 0) >= v:
                continue
            if need.get(k, 0) < v:
                need[k] = v
        for k, v in need.items():
            self.waited[eng][k] = v
        return list(need.items())

    def op(self, eng, fn, reads=(), writes=()):
        if not self.enabled:
            return None
        waits = self._waits(eng, self._deps(reads, writes, eng))
        self.seq[eng] += 1
        tok = (eng, self.seq[eng])
        self.ops[eng].append((waits, fn, (eng, 1)))
        self._commit(tok, reads, writes)
        return tok

    def dma(self, fn, reads=(), writes=(), q="sp"):
        if not self.enabled:
            return None
        i = self.dma_i % self.NDMA
        self.dma_i += 1
        deps = self._deps(reads, writes, q)
        if self.dma_tok[i] is not None:
            deps.append(self.dma_tok[i])
        waits = self._waits(q, deps)
        self.dma_val[i] += 16
        tok = (("dma", i), self.dma_val[i])
        self.dma_tok[i] = tok
        self.ops[q].append((waits, fn, (("dma", i), 16)))
        self._commit(tok, reads, writes)
        return tok

    def barrier(self):
        toks = [(e, self.seq[e]) for e in self.ENGS if self.seq[e] > 0]
        toks += [(("dma", i), self.dma_val[i]) for i in range(self.NDMA) if self.dma_val[i] > 0]
        for eng in self.ENGS:
            waits = self._waits(eng, toks)
            if waits:
                self.ops[eng].append((waits, None, None))

    def emit(self, final=False):
        nc = self.nc
        sems = self.sems
        self.barrier()
        if final:
            waits = self._waits("sp", list(self.out_toks))
            self.ops["sp"].append((waits, None, None))
        with nc.Block() as block:
            def run(engobj, name):
                for waits, fn, inc in self.ops[name]:
                    for k, v in waits:
                        engobj.wait_ge(sems[k], v)
                    if fn is None:
                        continue
                    ins = fn(engobj)
                    ins.then_inc(sems[inc[0]], inc[1])
                self.ops[name] = []

            @block.tensor
            def _(e):
                run(e, "pe")

            @block.scalar
            def _(e):
                run(e, "act")

            @block.vector
            def _(e):
                run(e, "dve")

            @block.gpsimd
            def _(e):
                run(e, "pool")

            @block.sync
            def _(e):
                run(e, "sp")


def build_nc():
    nc = bass.Bass("TRN2", target_bir_lowering=False)

    def DR(name, shape, dt=F32, kind="ExternalInput"):
        return nc.dram_tensor(name, shape, dt, kind=kind).ap()

    xTf = DR("xTf", [DM, S]); xTo = DR("xTo", [DM, 2048]); xo = DR("xo", [2048, DM])
    w_kvf = DR("w_kvf", [DM, 640]); w_vt = DR("w_vt", [DM, 384]); w_q = DR("w_q", [DM, 1024])
    w_gn = DR("w_gn", [DM, 24]); w_gm = DR("w_gm", [DM, 2048])
    ropef = DR("ropef", [128, 2, S]); ropeo = DR("ropeo", [128, NJ, 2, 512])
    pe2 = DR("pe2", [128, 2, 16]); cw1 = DR("cw1", [2, 2048, 128]); cw2 = DR("cw2", [128, 2, 64])
    cmaskT = DR("cmaskT", [128, NJ, 4, 512], BF16); AB = DR("AB", [128, NJ, 2, 128])
    validm = DR("validm", [128, 128]); ovl = DR("ovl", [128, 4, 128], BF16); indD = DR("ind", [64, S], BF16)
    constsD = DR("consts", [128, 9, 128], BF16); mb4D = DR("mb4", [128, 6, 512], BF16); identfD = DR("identf", [128, 128])
    sinksD = DR("sinks", [128, 8]); wbr = DR("wbr", [2, 512, DM]); wout = DR("wout", [DM, DM])
    lnD = DR("ln", [128, 4, DM]); wrD = DR("wr", [DM, 32]); brD = DR("br", [128, 32]); ecapD = DR("ecap", [128, 32])
    wei = DR("wei", [32, DM, 2048]); beiD = DR("bei", [128, 32, 16]); weo = DR("weo", [32, DM, DM]); beoD = DR("beo", [32, DM])
    yout = DR("y", [2048, DM], F32, "ExternalOutput")
    gmb = DR("gmb", [2048, 2048], BF16, "Internal")
    hbuf = DR("hbuf", [2048, DM], F32, "Internal")
    Xbuf = DR("Xbuf", [NSLOT + 128, DM], BF16, "Internal")
    Ybuf = DR("Ybuf", [NSLOT + 128, DM], F32, "Internal")
    obuf = DR("obuf", [2048, DM], BF16, "Internal")

    with contextlib.ExitStack() as top:
        sems = {}
        for e in Sched.ENGS:
            sems[e] = top.enter_context(nc.semaphore("s_" + e))
        for i in range(Sched.NDMA):
            sems[("dma", i)] = top.enter_context(nc.semaphore("s_dma%d" % i))
        s = Sched(nc, sems)

        def TT(stack, name, shape, dt):
            return stack.enter_context(nc.sbuf_tensor("sb_" + name, shape, dt))

        pb = [top.enter_context(nc.psum_tensor("pb%d" % i, [128, 512], F32)) for i in range(8)]
        PB = ["pb%d" % i for i in range(8)]

        def mm(out, lhsT, rhs, start, stop, reads, writes):
            s.op("pe", lambda e: e.matmul(out, lhsT=lhsT, rhs=rhs, start=start, stop=stop, skip_group_check=True), reads, writes)

        def tr(out, in_, ident, reads, writes):
            s.op("pe", lambda e: e.transpose(out, in_, ident), reads, writes)

        def cp(eng, out, in_, reads, writes):
            if eng == "act":
                s.op("act", lambda e: e.copy(out, in_), reads, writes)
            else:
                s.op(eng, lambda e: e.tensor_copy(out, in_), reads, writes)

        def tt(eng, out, in0, in1, op, reads, writes):
            s.op(eng, lambda e: e.tensor_tensor(out, in0, in1, op), reads, writes)

        def ts(eng, out, in0, s1, op0, reads, writes, s2=None, op1=None):
            if op1 is None:
                s.op(eng, lambda e: e.tensor_scalar(out, in0, s1, None, op0), reads, writes)
            else:
                s.op(eng, lambda e: e.tensor_scalar(out, in0, s1, s2, op0, op1), reads, writes)

        def stt(eng, out, in0, sc, in1, op0, op1, reads, writes):
            s.op(eng, lambda e: e.scalar_tensor_tensor(out, in0, sc, in1, op0, op1), reads, writes)

        def act(out, in_, func, reads, writes, bias=0.0, scale=1.0):
            s.op("act", lambda e: e.activation(out, in_, func, bias=bias, scale=scale), reads, writes)

        def dma(out, in_, reads, writes, q="sp"):
            return s.dma(lambda e: e.dma_start(out=out, in_=in_), reads, writes, q=q)

        consts = TT(top, "consts", [128, 9, 128], BF16)
        identf = TT(top, "identf", [128, 128], F32)
        stg = [TT(top, "stg%d" % i, [128, 2048], F32) for i in range(3)]
        nstg = {"n": 3}
        dma(consts[:], constsD, [], ["consts"])
        dma(identf[:], identfD, [], ["identf"])
        C_CAUSAL, C_ANTI, C_W0, C_W1, C_W2, C_S2, C_TRIU, C_ONES, C_ID = range(9)
        ident_bf = consts[:, C_ID, :]
        st_state = {"i": 0, "c": 0}
        cast_engs = ["pool", "act", "dve", "pool", "act"]

        def load_cast(dst, src, nelem, view, dkey, eng=None):
            i = st_state["i"] % nstg["n"]
            st_state["i"] += 1
            sv = view(stg[i][:, 0:nelem])
            dma(sv, src, [], ["stg%d" % i])
            if eng is None:
                eng = cast_engs[st_state["c"] % len(cast_engs)]
                st_state["c"] += 1
            cp(eng, dst, sv, ["stg%d" % i], [dkey])

        def lc_dma(src, nelem, view):
            i = st_state["i"] % nstg["n"]
            st_state["i"] += 1
            sv = view(stg[i][:, 0:nelem])
            dma(sv, src, [], ["stg%d" % i])
            return (sv, i)

        def lc_cast(h, dst, dkey, eng):
            cp(eng, dst, h[0], ["stg%d" % h[1]], [dkey])

        with contextlib.ExitStack() as ab:
            KI = [TT(ab, "KI%d" % g, [128, S], BF16) for g in range(2)]
            kwT = TT(ab, "kwT", [128, S], BF16)
            kswT = TT(ab, "kswT", [128, S // 2], BF16)
            vs_aug = TT(ab, "vs_aug", [128, 64, 2, 65], BF16)
            vw_aug = TT(ab, "vw_aug", [128, 64, 2, 65], BF16)
            vsw_aug = TT(ab, "vsw_aug", [128, 32, 2, 65], BF16)
            kcT = TT(ab, "kcT", [128, 512], BF16)
            vc_aug = TT(ab, "vc_aug", [128, 4, 2, 193], BF16)

            xTo_v = xTo.rearrange("(dc p) t -> p dc t", p=128)

            def load_xoj(dst, j, key):
                load_cast(dst[:, :, :], xTo_v[:, :, j * 128:(j + 1) * 128], 1024,
                          lambda a: a.rearrange("p (a b) -> p a b", a=8), key)

            def xoj_dma(j):
                return lc_dma(xTo_v[:, :, j * 128:(j + 1) * 128], 1024, lambda a: a.rearrange("p (a b) -> p a b", a=8))

            s.enabled = False
            with contextlib.ExitStack() as ph:
                wgm_bf = TT(ph, "wgm_bf", [128, 8, 2048], BF16)
                sg = [TT(ph, "sg%d" % i, [128, 2048], BF16) for i in range(2)]
                wgm_v = w_gm.rearrange("(dc p) c -> p dc c", p=128)
                xoj = [TT(ph, "xojA%d" % i, [128, 8, 128], BF16) for i in range(2)]
                for i in range(8):
                    load_cast(wgm_bf[:, i, :], wgm_v[:, i, :], 2048, lambda a: a, "wgm_bf")
                load_xoj(xoj[0], 0, "xoj0")
                for j in range(NJ):
                    sl = j % 2
                    xh = xoj_dma(j + 1) if j + 1 < NJ else None
                    for cc in range(4):
                        if cc == 2 and xh is not None:
                            lc_cast(xh, xoj[1 - sl][:, :, :], "xoj%d" % (1 - sl), "dve")
                        b = cc % 4
                        for dc in range(8):
                            mm(pb[b][:, :], xoj[sl][:, dc, :], wgm_bf[:, dc, cc * 512:(cc + 1) * 512],
                               dc == 0, dc == 7, ["xoj%d" % sl, "wgm_bf"], [PB[b]])
                        act(sg[sl][:, cc * 512:(cc + 1) * 512], pb[b][:, :], AF.Sigmoid, [PB[b]], ["sg%d_%d" % (sl, cc)])
                    dma(gmb[j * 128:(j + 1) * 128, :], sg[sl][:], ["sg%d_%d" % (sl, c) for c in range(4)], ["gmb%d" % j])
                s.emit()

            s.enabled = _LV >= 1
            with contextlib.ExitStack() as ph:
                wkvf_bf = TT(ph, "wkvf_bf", [128, 8, 640], BF16)
                wvt_bf = TT(ph, "wvt_bf", [128, 8, 384], BF16)
                cw1_bf = TT(ph, "cw1_bf", [128, 2, 16, 128], BF16)
                cw2_bf = TT(ph, "cw2_bf", [128, 2, 64], BF16)
                pe_bf = TT(ph, "pe_bf", [128, 2, 16], BF16)
                cbias = TT(ph, "cbias", [128, 2], F32)
                xt_bf = [TT(ph, "xt_bf%d" % i, [128, 8, 512], BF16) for i in range(2)]
                ropet = [TT(ph, "ropet%d" % i, [128, 2, 512], F32) for i in range(2)]
                cb = [[TT(ph, "cb%d_%d" % (i, k), [128, 528], BF16) for k in range(4)] for i in range(2)]
                tmpA = [TT(ph, "tmpA%d" % i, [128, 512], F32) for i in range(2)]
                tmpB = [TT(ph, "tmpB%d" % i, [128, 512], F32) for i in range(2)]
                gx = TT(ph, "gx", [128, 128], F32)
                gy = TT(ph, "gy", [128, 128], F32)
                gz = TT(ph, "gz", [128, 128], F32)
                gT = TT(ph, "gT", [128, 128], BF16)

                wk_v = w_kvf.rearrange("(dc p) c -> p dc c", p=128)
                for i in range(4):
                    load_cast(wkvf_bf[:, 2 * i:2 * i + 2, :], wk_v[:, 2 * i:2 * i + 2, :], 1280,
                              lambda a: a.rearrange("p (a b) -> p a b", a=2), "wkvf_bf")
                wvt_v = w_vt.rearrange("(dc p) c -> p dc c", p=128)
                for i in range(2):
                    load_cast(wvt_bf[:, 4 * i:4 * i + 4, :], wvt_v[:, 4 * i:4 * i + 4, :], 1536,
                              lambda a: a.rearrange("p (a b) -> p a b", a=4), "wvt_bf")
                cw1_v = cw1.rearrange("a (lp p) m -> p a lp m", p=128)
                for a_ in range(2):
                    load_cast(cw1_bf[:, a_, :, :], cw1_v[:, a_, :, :], 2048,
                              lambda a: a.rearrange("p (a b) -> p a b", a=16), "cw1_bf")
                load_cast(cw2_bf[:], cw2, 128, lambda a: a.rearrange("p (a b) -> p a b", a=2), "cw2_bf")
                load_cast(pe_bf[:], pe2, 32, lambda a: a.rearrange("p (a b) -> p a b", a=2), "pe_bf")
                for nm, vt in (("vs_aug", vs_aug), ("vw_aug", vw_aug), ("vsw_aug", vsw_aug)):
                    s.op("pool", (lambda t: (lambda e: e.memset(t[:, :, :, 64:65], 1.0)))(vt), [], [nm + "_one"])
                s.op("pool", lambda e: e.memset(vc_aug[:, :, :, 64:65], 1.0), [], ["vc_one"])
                for g in range(2):
                    dma(vc_aug[:, :, g, 65:193], ovl, [], ["vc_ovl%d" % g])
                for i in range(2):
                    for k in range(4):
                        s.op("pool", (lambda t: (lambda e: e.memset(t[:, :], 0.0)))(cb[i][k]), [], ["cb%d_%d" % (i, k)])
                for g in range(2):
                    dma(KI[g][64:128, :], indD, [], ["KIind%d" % g])
                for a_ in range(2):
                    for lp in range(16):
                        mm(pb[6][:, a_:a_ + 1], cw1_bf[:, a_, lp, :], pe_bf[:, a_, lp:lp + 1], lp == 0, lp == 15,
                           ["cw1_bf", "pe_bf"], [PB[6]])
                cp("dve", cbias[:, :], pb[6][:, 0:2], [PB[6]], ["cbias"])

                xTf_v = xTf.rearrange("(dc p) t -> p dc t", p=128)

                def rope_to(dst, ps, tab, ti, n, rkeys, wkeys, psk, split=None):
                    tt("dve", tmpA[ti][:, 0:n], ps, tab[:, 0, :], ALU.mult, [psk] + rkeys, ["tmpA%d" % ti])
                    for (o0, i0) in ((0, 32), (32, 0), (64, 96), (96, 64)):
                        tt("dve", tmpB[ti][o0:o0 + 32, 0:n], ps[i0:i0 + 32, :], tab[o0:o0 + 32, 1, :], ALU.mult,
                           [psk] + rkeys, ["tmpB%d_%d" % (ti, o0)])
                    if split is not None:
                        for g_ in range(2):
                            tt("pool", split[g_], tmpA[ti][64 * g_:64 * g_ + 64, 0:n], tmpB[ti][64 * g_:64 * g_ + 64, 0:n], ALU.add,
                               ["tmpA%d" % ti] + ["tmpB%d_%d" % (ti, o) for o in (0, 32, 64, 96)], wkeys)
                        return
                    tt("pool", dst, tmpA[ti][:, 0:n], tmpB[ti][:, 0:n], ALU.add,
                       ["tmpA%d" % ti] + ["tmpB%d_%d" % (ti, o) for o in (0, 32, 64, 96)], wkeys)

                def xt_dma(ti_):
                    return [lc_dma(xTf_v[:, 4 * hf:4 * hf + 4, ti_ * 512:(ti_ + 1) * 512], 2048,
                                   lambda a: a.rearrange("p (a b) -> p a b", a=4)) for hf in range(2)]

                def xt_cast(hs_, sl_):
                    for hf in range(2):
                        lc_cast(hs_[hf], xt_bf[sl_][:, 4 * hf:4 * hf + 4, :], "xt_bf%d" % sl_, "act" if hf == 0 else "dve")

                xt_cast(xt_dma(0), 0)
                dma(ropet[0][:], ropef[:, :, 0:512], [], ["ropet0"])
                for tt_i in range(16):
                    sl = tt_i % 2
                    t0 = tt_i * 512
                    xth = None
                    if tt_i + 1 < 16:
                        xth = xt_dma(tt_i + 1)
                        dma(ropet[1 - sl][:], ropef[:, :, t0 + 512:t0 + 1024], [], ["ropet%d" % (1 - sl)])
                    for fc in range(5):
                        if fc == 3 and xth is not None:
                            xt_cast(xth, 1 - sl)
                        b = fc % 2
                        for dc in range(8):
                            mm(pb[b][:, :], wkvf_bf[:, dc, fc * 128:(fc + 1) * 128], xt_bf[sl][:, dc, :], dc == 0, dc == 7,
                               ["wkvf_bf", "xt_bf%d" % sl], [PB[b]])
                        if fc < 2:
                            for g in range(2):
                                k = fc * 2 + g
                                ck = "cb%d_%d" % (sl, k)
                                cp("act", cb[sl][k][0:64, 16:528], pb[b][64 * g:64 * g + 64, :], [PB[b]], [ck])
                                cp("dve", cb[sl][k][64:128, 15:527], pb[b][64 * g:64 * g + 64, :], [PB[b]], [ck])
                        else:
                            if fc == 2:
                                rope_to(None, pb[b][:, :], ropet[sl], fc % 2, 512, ["ropet%d" % sl], ["KI"], PB[b],
                                        split=[KI[0][0:64, t0:t0 + 512], KI[1][0:64, t0:t0 + 512]])
                            elif fc == 3:
                                rope_to(kwT[:, t0:t0 + 512], pb[b][:, :], ropet[sl], fc % 2, 512, ["ropet%d" % sl], ["kwT"], PB[b])
                            else:
                                rope_to(kswT[:, tt_i * 256:(tt_i + 1) * 256], pb[b][:, 256:512], ropet[sl][:, :, 256:512], fc % 2, 256,
                                        ["ropet%d" % sl], ["kswT"], PB[b])
                    for ti in range(4):
                        b = 2 + ti % 2
                        for dc in range(8):
                            mm(pb[b][:, 0:384], xt_bf[sl][:, dc, ti * 128:(ti + 1) * 128], wvt_bf[:, dc, :], dc == 0, dc == 7,
                               ["xt_bf%d" % sl, "wvt_bf"], [PB[b]])
                        for vi, (nm, vt) in enumerate((("vs_aug", vs_aug), ("vw_aug", vw_aug), ("vsw_aug", vsw_aug))):
                            if vi == 2 and ti < 2:
                                continue
                            tix = (2 * tt_i + ti - 2) if vi == 2 else (4 * tt_i + ti)
                            cp("act", vt[:, tix, :, 0:64],
                               pb[b][:, vi * 128:(vi + 1) * 128].rearrange("p (g d) -> p g d", g=2), [PB[b]], [nm])
                    if tt_i > 0:
                        for k in range(4):
                            ck = "cb%d_%d" % (sl, k)
                            pk = "cb%d_%d" % (1 - sl, k)
                            cp("pool", cb[sl][k][0:64, 0:16], cb[1 - sl][k][0:64, 512:528], [pk], [ck])
                            cp("pool", cb[sl][k][64:128, 0:15], cb[1 - sl][k][64:128, 512:527], [pk], [ck])
                    for k in range(4):
                        a_ = k // 2
                        for lp in range(16):
                            mm(pb[4][:, k * 32:(k + 1) * 32], cw1_bf[:, a_, lp, :], cb[sl][k][:, 2 * lp:2 * lp + 497:16],
                               lp == 0, lp == 15, ["cw1_bf", "cb%d_%d" % (sl, k)], [PB[4]])
                    for a_ in range(2):
                        act(gx[:, a_ * 64:(a_ + 1) * 64], pb[4][:, a_ * 64:(a_ + 1) * 64], AF.Identity, [PB[4], "cbias"], ["gx%d" % a_],
                            bias=cbias[:, a_:a_ + 1])
                    tt("dve", gy[:, :], gx[:, :], gx[:, :], ALU.mult, ["gx0", "gx1"], ["gy"])
                    ts("dve", gy[:, :], gy[:, :], 0.044715, ALU.mult, ["gy"], ["gy"], 1.0, ALU.add)
                    tt("dve", gy[:, :], gy[:, :], gx[:, :], ALU.mult, ["gy", "gx0", "gx1"], ["gy"])
                    act(gz[:, :], gy[:, :], AF.Sigmoid, ["gy"], ["gz"], scale=1.5957691216057308)
                    tt("dve", gT[:, :], gz[:, :], gx[:, :], ALU.mult, ["gz", "gx0", "gx1"], ["gT"])
                    for g in range(2):
                        mm(pb[5][0:64, g * 32:(g + 1) * 32], cw2_bf[:, 0, :], gT[:, g * 32:(g + 1) * 32], True, True,
                           ["cw2_bf", "gT"], [PB[5]])
                    for g in range(2):
                        mm(pb[5][0:32, 64 + g * 64:128 + g * 64], gT[:, 64 + g * 32:96 + g * 32], cw2_bf[:, 1, :], True, True,
                           ["cw2_bf", "gT"], [PB[5]])
                    for g in range(2):
                        cp("dve", kcT[64 * g:64 * g + 64, tt_i * 32:(tt_i + 1) * 32], pb[5][0:64, g * 32:(g + 1) * 32], [PB[5]], ["kcT"])
                        p0 = 32 * (tt_i % 4)
                        cp("act", vc_aug[p0:p0 + 32, tt_i // 4, g, 0:64], pb[5][0:32, 64 + g * 64:128 + g * 64], [PB[5]], ["vc_aug"])
                s.emit()

            s.enabled = _LV >= 2

            with contextlib.ExitStack() as ph:
                wq_bf = TT(ph, "wq_bf", [128, 8, 1024], BF16)
                wgn_bf = TT(ph, "wgn_bf", [128, 8, 24], BF16)
                QS = [[TT(ph, "QS%d_%d" % (g, hf), [128, 4, 128], BF16) for hf in range(2)] for g in range(2)]
                ABtL = [TT(ph, "ABt%d" % i, [128, 2, 128], F32) for i in range(2)]
                vmt = TT(ph, "vmt", [128, 128], F32)
                cmtL = [TT(ph, "cmt%d" % i, [128, 4, 512], BF16) for i in range(2)]
                ropejL = [TT(ph, "ropej%d" % i, [128, 2, 512], F32) for i in range(2)]
                jk = {}
                esink = TT(ph, "esink", [128, 8], F32)
                qnu = TT(ph, "qnu", [128, 4, 128], BF16)
                qnr = TT(ph, "qnr", [128, 4, 128], BF16)
                qsr = TT(ph, "qsr", [128, 4, 128], BF16)
                tmpA = [TT(ph, "tmpA", [128, 512], F32)]
                tmpB = [TT(ph, "tmpB", [128, 512], F32)]
                gn = TT(ph, "gn", [128, 24], F32)
                imp = TT(ph, "imp", [128, 128], F32)
                imw = TT(ph, "imw", [128, 128], F32)
                m8 = TT(ph, "m8", [128, 16], F32)
                o_f = TT(ph, "o_f", [128, 1024], F32)
                o_bf = TT(ph, "o_bf", [128, 1024], BF16)

                wq_v = w_q.rearrange("(dc p) c -> p dc c", p=128)
                for i in range(4):
                    load_cast(wq_bf[:, 2 * i:2 * i + 2, :], wq_v[:, 2 * i:2 * i + 2, :], 2048,
                              lambda a: a.rearrange("p (a b) -> p a b", a=2), "wq_bf")
                load_cast(wgn_bf[:], w_gn.rearrange("(dc p) c -> p dc c", p=128), 192,
                          lambda a: a.rearrange("p (a b) -> p a b", a=8), "wgn_bf")
                xoj = [TT(ph, "xojB%d" % i, [128, 8, 128], BF16) for i in range(2)]
                dma(vmt[:], validm, [], ["vmt"])
                dma(esink[:], sinksD, [], ["esink"])
                act(esink[:, :], esink[:, :], AF.Exp, ["esink"], ["esink"])

                def rope_to(dst, ps, tab, n, rkeys, wkeys, psk):
                    tt("dve", tmpA[0][:, 0:n], ps, tab[:, 0, :], ALU.mult, [psk] + rkeys, ["tmpA"])
                    for (o0, i0) in ((0, 32), (32, 0), (64, 96), (96, 64)):
                        tt("dve", tmpB[0][o0:o0 + 32, 0:n], ps[i0:i0 + 32, :], tab[o0:o0 + 32, 1, :], ALU.mult,
                           [psk] + rkeys, ["tmpB_%d" % o0])
                    tt("dve", dst, tmpA[0][:, 0:n], tmpB[0][:, 0:n], ALU.add,
                       ["tmpA"] + ["tmpB_%d" % o for o in (0, 32, 64, 96)], wkeys)

                NEG = 30000.0
                mb4 = TT(ph, "mb4", [128, 6, 512], BF16)
                dma(mb4[:], mb4D, [], ["mb4"])
                M_CAUSAL, M_ANTI, M_W0, M_W1, M_W2, M_S2 = range(6)
                negq = [TT(ph, "negq%d" % g, [128, 128], BF16) for g in range(2)]
                Et3 = [TT(ph, "Et3_%d" % i, [128, 4, 128], BF16) for i in range(3)]
                o_cmp = TT(ph, "o_cmp", [128, 512], F32)
                o_sel = TT(ph, "o_sel", [128, 512], F32)
                o_win = TT(ph, "o_win", [128, 512], F32)
                den2 = [TT(ph, "den2_%d" % i, [128, 4], F32) for i in range(8)]
                coef2 = [TT(ph, "coef2_%d" % i, [128, 4], F32) for i in range(8)]
                pipe = {"i": 0, "pending": None}

                def flush():
                    p = pipe["pending"]
                    if p is None:
                        return
                    pipe["pending"] = None
                    (eb, vaug, vkey, kt, g, accs, first, last, post) = p
                    for hh in range(4):
                        ab_, col, ncol = accs[hh]
                        st_flag = first and (hh == 0 or accs[hh][0] != accs[hh - 1][0])
                        mm(pb[ab_][:, col:col + ncol], Et3[eb][:, hh, :], vaug[:, kt, g, :], st_flag, last and hh == 3,
                           ["Et3_%d" % eb, vkey], [PB[ab_]])
                    if last and post is not None:
                        post()

                def tile(kT, kkey, vaug, vkey, kt, g, q, qkey, accs, first, last, biases, post=None, fullk=False):
                    i = pipe["i"]
                    pipe["i"] += 1
                    sb = i % 2
                    eb = i % 3
                    nb = len(biases)
                    if fullk:
                        mm(pb[sb][:, :], kT[:, kt * 128:(kt + 1) * 128], q[:, :, :], True, nb == 0, kkey + qkey, [PB[sb]])
                    else:
                        mm(pb[sb][:, :], kT[64 * g:64 * g + 64, kt * 128:(kt + 1) * 128], q[64 * g:64 * g + 64, :, :], True, nb == 0,
                           [kkey, qkey], [PB[sb]])
                    for bi, (bl, br_, bk) in enumerate(biases):
                        mm(pb[sb][:, :], bl, br_, False, bi == nb - 1, bk, [PB[sb]])
                    flush()
                    act(Et3[eb][:, :, :], pb[sb][:, :].rearrange("p (h q) -> p h q", h=4), AF.Exp, [PB[sb]], ["Et3_%d" % eb], scale=0.125)
                    pipe["pending"] = (eb, vaug, vkey, kt, g, accs, first, last, post)

                def bias_const(mi):
                    return (ident_bf, mb4[:, mi, :], ["consts", "mb4"])

                def acc65(bank):
                    return [(bank, hh * 65, 65) for hh in range(4)]

                def finish_branch(bank, g, gate_off, dst, dkey_pfx, slot, sink=False):
                    d, c = den2[slot], coef2[slot]
                    dk, ck = "den2_%d" % slot, "coef2_%d" % slot
                    dview = pb[bank][:, 0:260].rearrange("p (h c) -> p h c", c=65)[:, :, 64:65]
                    if sink:
                        tt("dve", d[:, :].rearrange("p (h o) -> p h o", o=1), dview,
                           esink[:, 4 * g:4 * g + 4].rearrange("p (h o) -> p h o", o=1), ALU.add, [PB[bank], "esink"], [dk])
                    else:
                        cp("dve", d[:, :].rearrange("p (h o) -> p h o", o=1), dview, [PB[bank]], [dk])
                    s.op("dve", lambda e: e.reciprocal(d[:, :], d[:, :]), [dk], [dk])
                    for hh in range(4):
                        h = 4 * g + hh
                        if gate_off is None:
                            sc, sk = d[:, hh:hh + 1], dk
                        else:
                            tt("dve", c[:, hh:hh + 1], d[:, hh:hh + 1], gn[:, 3 * h + gate_off:3 * h + gate_off + 1], ALU.mult, [dk, "gn"], [ck + "_%d" % hh])
                            sc, sk = c[:, hh:hh + 1], ck + "_%d" % hh
                        ts("dve", dst[:, h * 64:(h + 1) * 64], pb[bank][:, hh * 65:hh * 65 + 64], sc, ALU.mult,
                           [PB[bank], sk], [dkey_pfx + "%d" % h])

                def finish_cmp(banks, g, slot):
                    d, c = den2[slot], coef2[slot]
                    dk, ck = "den2_%d" % slot, "coef2_%d" % slot
                    for bk in range(2):
                        cp("dve", d[:, 2 * bk:2 * bk + 2].rearrange("p (h o) -> p h o", o=1),
                           pb[banks[bk]][:, :].rearrange("p (h c) -> p h c", c=256)[:, :, 64:65], [PB[banks[bk]]], [dk])
                    ts("dve", d[:, :], d[:, :], 1e-30, ALU.max, [dk], [dk])
                    s.op("dve", lambda e: e.reciprocal(d[:, :], d[:, :]), [dk], [dk])
                    for hh in range(4):
                        bk, col = banks[hh // 2], (hh % 2) * 256
                        if hh == 0:
                            ts("dve", imp[:, :], pb[bk][:, col + 65:col + 193], d[:, hh:hh + 1], ALU.mult, [PB[bk], dk], ["imp"])
                        else:
                            stt("dve", imp[:, :], pb[bk][:, col + 65:col + 193], d[:, hh:hh + 1], imp[:, :], ALU.mult, ALU.add,
                                [PB[bk], dk, "imp"], ["imp"])
                    for hh in range(4):
                        h = 4 * g + hh
                        bk, col = banks[hh // 2], (hh % 2) * 256
                        tt("dve", c[:, hh:hh + 1], d[:, hh:hh + 1], gn[:, 3 * h:3 * h + 1], ALU.mult, [dk, "gn"], [ck + "_%d" % hh])
                        ts("dve", o_cmp[:, h * 64:(h + 1) * 64], pb[bk][:, col:col + 64], c[:, hh:hh + 1], ALU.mult,
                           [PB[bk], ck + "_%d" % hh], ["o_cmp%d" % h])
                    tt("dve", imp[:, :], imp[:, :], ABt[:, 0, :], ALU.mult, ["imp", jk["ABt"]], ["imp"])
                    tt("dve", imp[:, :], imp[:, :], ABt[:, 1, :], ALU.add, ["imp", jk["ABt"]], ["imp"])
                    s.op("dve", lambda e: e.max(out=m8[:, 0:8], in_=imp[:, :]), ["imp"], ["m8a"])
                    s.op("dve", lambda e: e.match_replace(out=imw[:, :], in_to_replace=m8[:, 0:8], in_values=imp[:, :], imm_value=-1e30),
                         ["imp", "m8a"], ["imw"])
                    s.op("dve", lambda e: e.max(out=m8[:, 8:16], in_=imw[:, :]), ["imw"], ["m8b"])
                    ts("dve", imw[:, :], imp[:, :], m8[:, 15:16], ALU.is_ge, ["imp", "m8b", "imw"], ["imw"])
                    tt("dve", imw[:, :], imw[:, :], vmt[:, :], ALU.mult, ["imw", "vmt"], ["imw"])
                    ts("dve", negq[g][:, :], imw[:, :], NEG, ALU.mult, ["imw"], ["negq%d" % g], -NEG, ALU.add)

                load_xoj(xoj[0], 0, "xoj0")
                for j in range(NJ):
                    jb = slice(j * 128, (j + 1) * 128)
                    xs = j % 2
                    xh = xoj_dma(j + 1) if j + 1 < NJ else None
                    if j == 0:
                        dma(ropejL[0][:], ropeo[:, 0, :, :], [], ["ropej0"])
                        dma(cmtL[0][:], cmaskT[:, 0, :, :], [], ["cmt0"])
                        dma(ABtL[0][:], AB[:, 0, :, :], [], ["ABt0"])
                    if j + 1 < NJ:
                        dma(ropejL[1 - xs][:], ropeo[:, j + 1, :, :], [], ["ropej%d" % (1 - xs)])
                        dma(cmtL[1 - xs][:], cmaskT[:, j + 1, :, :], [], ["cmt%d" % (1 - xs)])
                        dma(ABtL[1 - xs][:], AB[:, j + 1, :, :], [], ["ABt%d" % (1 - xs)])
                    ropej, cmt, ABt = ropejL[xs], cmtL[xs], ABtL[xs]
                    jk["ABt"], jk["cmt"], jk["ropej"] = "ABt%d" % xs, "cmt%d" % xs, "ropej%d" % xs
                    for qc in range(8):
                        b = 4 + qc // 4
                        for dc in range(8):
                            mm(pb[b][:, (qc % 4) * 128:(qc % 4 + 1) * 128], wq_bf[:, dc, qc * 128:(qc + 1) * 128], xoj[xs][:, dc, :],
                               dc == 0, dc == 7, ["wq_bf", "xoj%d" % xs], [PB[b]])
                    cp("act", qnu[:, :, :], pb[4][:, :].rearrange("p (h q) -> p h q", h=4), [PB[4]], ["qnu"])
                    rope_to(qnr[:, :, :].rearrange("p h q -> p (h q)"), pb[4][:, :], ropej, 512, [jk["ropej"]], ["qnr"], PB[4])
                    rope_to(qsr[:, :, :].rearrange("p h q -> p (h q)"), pb[5][:, :], ropej, 512, [jk["ropej"]], ["qsr"], PB[5])
                    nhalf = 1 if 4 * j + 4 <= 32 else 2
                    for g in range(2):
                        for hf in range(nhalf):
                            cp("pool", QS[g][hf][0:64, :, :], qnr[64 * g:64 * g + 64, :, :], ["qnr"], ["QSq%d_%d" % (g, hf)])
                    for dc in range(8):
                        mm(pb[2][:, 0:24], xoj[xs][:, dc, :], wgn_bf[:, dc, :], dc == 0, dc == 7, ["xoj%d" % xs, "wgn_bf"], [PB[2]])
                    act(gn[:, :], pb[2][:, 0:24], AF.Sigmoid, [PB[2]], ["gn"])
                    nct = min(4, (32 * j + 32 + 127) // 128)
                    for g in range(2):
                        banks = (4, 5) if g == 0 else (6, 7)
                        accs = [(banks[hh // 2], (hh % 2) * 256, 193) for hh in range(4)]
                        for ct in range(nct):
                            tile(kcT, "kcT", vc_aug, "vc_aug", ct, g, qnu, "qnu", accs, ct == 0, ct == nct - 1,
                                 [(ident_bf, cmt[:, ct, :], ["consts", jk["cmt"]])],
                                 post=(lambda g_=g, banks_=banks: finish_cmp(banks_, g_, g_)))
                    for g in range(2):
                        wbank, sbank = (3, 2) if g == 0 else (4, 5)
                        tiles = [t_ for t_ in range(4 * j - 1, 4 * j + 4) if t_ >= 0]
                        for ii, kt in enumerate(tiles):
                            if j == 0:
                                bl = [bias_const((M_W0, M_W1, M_W2, M_CAUSAL)[kt])]
                            elif kt == 4 * j - 1:
                                bl = [bias_const(M_ANTI)]
                            elif kt == 4 * j + 3:
                                bl = [bias_const(M_CAUSAL)]
                            else:
                                bl = []
                            tile(kwT, "kwT", vw_aug, "vw_aug", kt, g, qnr, "qnr", acc65(wbank), ii == 0, ii == len(tiles) - 1, bl,
                                 post=(lambda g_=g, b_=wbank: finish_branch(b_, g_, 2, o_win, "o_win", 2 + g_)))
                        for ii, kt in enumerate((2 * j, 2 * j + 1)):
                            bl = [bias_const(M_CAUSAL if ii == 1 else (M_S2 if j == 0 else M_ANTI))]
                            tile(kswT, "kswT", vsw_aug, "vsw_aug", kt, g, qsr, "qsr", acc65(sbank), ii == 0, ii == 1, bl,
                                 post=(lambda g_=g, b_=sbank: finish_branch(b_, g_, None, o_f[:, 512:1024], "o_s", 4 + g_, sink=True)))
                    flush()
                    if xh is not None:
                        lc_cast(xh, xoj[1 - xs][:, :, :], "xoj%d" % (1 - xs), "pool")
                    for g in range(2):
                        tr(pb[2][:, :].bitcast(BF16)[:, 0:128], negq[g][:, :], ident_bf, ["negq%d" % g, "consts"], [PB[2]])
                        for hf in range(nhalf):
                            for hh in range(4):
                                cp("act" if hh % 2 == 0 else "dve", QS[g][hf][64:128, hh, :], pb[2][:, :].bitcast(BF16)[64 * hf:64 * hf + 64, 0:128],
                                   [PB[2]], ["QSn%d_%d_%d" % (g, hf, hh)])
                    nkt = 4 * j + 4
                    for g in range(2):
                        sbank = 6 + g
                        for kt in range(nkt):
                            hf = kt // 32
                            bl = [bias_const(M_CAUSAL)] if kt == nkt - 1 else []
                            qk = ["QSq%d_%d" % (g, hf)] + ["QSn%d_%d_%d" % (g, hf, hh) for hh in range(4)]
                            tile(KI[g], ["KI", "KIind%d" % g], vs_aug, "vs_aug", kt, g, QS[g][hf], qk, acc65(sbank), kt == 0, kt == nkt - 1, bl,
                                 post=(lambda g_=g, b_=sbank: finish_branch(b_, g_, 1, o_sel, "o_sel", 6 + g_)), fullk=True)
                    flush()
                    tt("dve", o_f[:, 0:512], o_cmp[:, :], o_sel[:, :], ALU.add,
                       ["o_cmp%d" % h for h in range(8)] + ["o_sel%d" % h for h in range(8)], ["o_fa"])
                    tt("dve", o_f[:, 0:512], o_f[:, 0:512], o_win[:, :], ALU.add, ["o_fa"] + ["o_win%d" % h for h in range(8)], ["o_fb"])
                    cp("dve", o_bf[:, :], o_f[:, :], ["o_fb"] + ["o_s%d" % h for h in range(8)], ["o_bf"])
                    dma(obuf[jb, :], o_bf[:, :], ["o_bf"], ["obuf%d" % j])
                s.emit()
        s.enabled = _LV >= 3
        mid = top.enter_context(contextlib.ExitStack())
        idx_all = TT(mid, "idx_all", [128, NJ, 4], I32)
        gates_all = TT(mid, "gates_all", [128, NJ, 4], F32)
        GT_all = TT(mid, "GT_all", [32, NJ, 128], F32)

        def layer_norm(src, dst, lnt, stats, mv, rstd, tmp, skey, dkey, pfx):
            for hf in range(2):
                s.op("dve", (lambda h_: (lambda e: e.bn_stats(stats[:, h_, :], src[:, h_ * 512:(h_ + 1) * 512])))(hf),
                     [skey], [pfx + "st%d" % hf])
            s.op("dve", lambda e: e.bn_aggr(mv[:, :], stats[:, :, :]), [pfx + "st0", pfx + "st1"], [pfx + "mv"])
            ts("dve", rstd[:, :], mv[:, 1:2], EPS, ALU.add, [pfx + "mv"], [pfx + "rs"])
            s.op("act", lambda e: e.sqrt(rstd[:, :], rstd[:, :]), [pfx + "rs"], [pfx + "rs"])
            s.op("dve", lambda e: e.reciprocal(rstd[:, :], rstd[:, :]), [pfx + "rs"], [pfx + "rs"])
            ts("dve", tmp[:, :], src[:, :], mv[:, 0:1], ALU.subtract, [skey, pfx + "mv", pfx + "rs"], [pfx + "tmp"], rstd[:, 0:1], ALU.mult)
            tt("dve", tmp[:, :], tmp[:, :], lnt[:, 0, :], ALU.mult, [pfx + "tmp", pfx + "lnt"], [pfx + "tmp"])
            tt("dve", dst, tmp[:, :], lnt[:, 1, :], ALU.add, [pfx + "tmp", pfx + "lnt"], [dkey])

        with contextlib.ExitStack() as ph:
            wbr_bf = TT(ph, "wbr_bf", [128, 2, 4, DM], BF16)
            wout_bf = TT(ph, "wout_bf", [128, 8, DM], BF16)
            wr_sb = TT(ph, "wr_sb", [128, 8, 32], F32)
            br_sb = TT(ph, "br_sb", [128, 32], F32)
            ecap = TT(ph, "ecap", [128, 32], F32)
            base_bc = TT(ph, "base_bc", [128, 32], F32)
            o_bf = TT(ph, "o_bf2", [128, 1024], BF16)
            lnt1 = TT(ph, "lnt1", [128, 2, DM], F32)
            dma(lnt1[:], lnD[:, 0:2, :], [], ["l1lnt"])
            oT = TT(ph, "oT", [128, 8, 128], BF16)
            sgm = TT(ph, "sgm", [128, 2048], BF16)
            t1 = TT(ph, "t1", [128, 1024], F32)
            t2 = TT(ph, "t2", [128, 1024], F32)
            mix_bf = TT(ph, "mix_bf", [128, 1024], BF16)
            mixT = TT(ph, "mixT", [128, 8, 128], BF16)
            xot = TT(ph, "xot", [128, DM], F32)
            u = TT(ph, "u", [128, DM], F32)
            hs = TT(ph, "hs", [128, DM], F32)
            h_bf = TT(ph, "h_bf", [128, DM], BF16)
            hT = TT(ph, "hT", [128, 8, 128], F32)
            stats = TT(ph, "stats", [128, 2, 6], F32)
            mv = TT(ph, "mv", [128, 2], F32)
            rstd = TT(ph, "rstd", [128, 1], F32)
            lg = TT(ph, "lg", [128, 32], F32)
            r8 = TT(ph, "r8", [128, 8], F32)
            Mf = TT(ph, "Mf", [128, 32], F32)
            Mb = TT(ph, "Mb", [128, 32], BF16)
            ex = TT(ph, "ex", [128, 32], F32)
            Gt = TT(ph, "Gt", [128, 32], F32)
            sm1 = TT(ph, "sm1", [128, 1], F32)
            rank = TT(ph, "rank", [128, 32], F32)
            vld = TT(ph, "vld", [128, 32], F32)
            slotm = TT(ph, "slotm", [128, 32], F32)
            s8 = TT(ph, "s8", [128, 8], F32)
            fix = TT(ph, "fix", [128, 4], F32)
            oh = TT(ph, "oh", [128, 32], F32)
            for m_ in range(2):
                wb_v = wbr[m_].rearrange("(ch p) c -> p ch c", p=128)
                for i in range(2):
                    load_cast(wbr_bf[:, m_, 2 * i:2 * i + 2, :], wb_v[:, 2 * i:2 * i + 2, :], 2048,
                              lambda a: a.rearrange("p (a b) -> p a b", a=2), "wbr_bf")
            wo_v = wout.rearrange("(ch p) c -> p ch c", p=128)
            for i in range(4):
                load_cast(wout_bf[:, 2 * i:2 * i + 2, :], wo_v[:, 2 * i:2 * i + 2, :], 2048,
                          lambda a: a.rearrange("p (a b) -> p a b", a=2), "wout_bf")
            dma(wr_sb[:], wrD.rearrange("(dc p) e -> p dc e", p=128), [], ["wr_sb"])
            dma(br_sb[:], brD, [], ["br_sb"])
            dma(ecap[:], ecapD, [], ["ecap"])
            s.op("dve", lambda e: e.memset(base_bc[:, :], 0.0), [], ["base_bc"])
            sgmL = [sgm, TT(ph, "sgm_b", [128, 2048], BF16)]
            wgm2 = TT(ph, "wgm2", [128, 8, 2048], BF16)
            wgm_v2 = w_gm.rearrange("(dc p) c -> p dc c", p=128)
            for i in range(8):
                load_cast(wgm2[:, i, :], wgm_v2[:, i, :], 2048, lambda a: a, "wgm2")
            xojC = [TT(ph, "xojC%d" % i, [128, 8, 128], BF16) for i in range(2)]
            load_xoj(xojC[0], 0, "xojC0")
            xotL = [xot, TT(ph, "xot_b", [128, DM], F32)]
            obfL = [o_bf, TT(ph, "o_bf2b", [128, 1024], BF16)]
            uL = [u, TT(ph, "u_b", [128, DM], F32)]
            lntmp = TT(ph, "lntmp", [128, DM], F32)

            xoj_handles = {}

            def stage1(j):
                jb = slice(j * 128, (j + 1) * 128)
                sl = j % 2
                sgm, xot, o_bf, u = sgmL[sl], xotL[sl], obfL[sl], uL[sl]
                sgk, xok, obk, uk = "sgm%d" % sl, "xot%d" % sl, "o_bf%d" % sl, "u%d" % sl
                xoj_handles[j] = xoj_dma(j + 1) if j + 1 < NJ else None
                for cc in range(4):
                    gb = (3, 7)[cc % 2]
                    for dc in range(8):
                        mm(pb[gb][:, :], xojC[sl][:, dc, :], wgm2[:, dc, cc * 512:(cc + 1) * 512], dc == 0, dc == 7,
                           ["xojC%d" % sl, "wgm2"], [PB[gb]])
                    act(sgm[:, cc * 512:(cc + 1) * 512], pb[gb][:, :], AF.Sigmoid, [PB[gb]], [sgk + "_%d" % cc])
                dma(xot[:], xo[jb, :], [], [xok])
                dma(o_bf[:, :], obuf[jb, :], ["obuf%d" % j], [obk])
                for ch in range(8):
                    tr(pb[6][:, :].bitcast(BF16)[:, ch * 128:(ch + 1) * 128], o_bf[:, ch * 128:(ch + 1) * 128], ident_bf,
                       [obk, "consts"], [PB[6]])
                cp("act", oT[:, :, :], pb[6][:, :].bitcast(BF16).rearrange("p (c q) -> p c q", c=8), [PB[6]], ["oT"])
                for m_ in range(2):
                    for hf in range(2):
                        b = 4 + hf if m_ == 0 else 6 + hf
                        for ch in range(4):
                            mm(pb[b][:, :], oT[:, 4 * m_ + ch, :], wbr_bf[:, m_, ch, hf * 512:(hf + 1) * 512], ch == 0, ch == 3,
                               ["oT", "wbr_bf"], [PB[b]])

            def stage1b(j):
                jb = slice(j * 128, (j + 1) * 128)
                sl = j % 2
                sgm, xot, o_bf, u = sgmL[sl], xotL[sl], obfL[sl], uL[sl]
                sgk, xok, obk, uk = "sgm%d" % sl, "xot%d" % sl, "o_bf%d" % sl, "u%d" % sl
                xh_ = xoj_handles.pop(j, None)
                for hf in range(2):
                    hs_ = slice(hf * 512, (hf + 1) * 512)
                    tt("dve", t1[:, hs_], pb[4 + hf][:, :], sgm[:, hf * 512:(hf + 1) * 512], ALU.mult, [PB[4 + hf], sgk + "_%d" % hf], ["t1_%d" % hf])
                    tt("dve", t2[:, hs_], pb[6 + hf][:, :], sgm[:, 1024 + hf * 512:1024 + (hf + 1) * 512], ALU.mult, [PB[6 + hf], sgk + "_%d" % (2 + hf)], ["t2_%d" % hf])
                tt("dve", mix_bf[:, :], t1[:, :], t2[:, :], ALU.add, ["t1_0", "t1_1", "t2_0", "t2_1"], ["mix_bf"])
                for ch in range(8):
                    tr(pb[6][:, :].bitcast(BF16)[:, ch * 128:(ch + 1) * 128], mix_bf[:, ch * 128:(ch + 1) * 128], ident_bf,
                       ["mix_bf", "consts"], [PB[6]])
                cp("act", mixT[:, :, :], pb[6][:, :].bitcast(BF16).rearrange("p (c q) -> p c q", c=8), [PB[6]], ["mixT"])
                for hf in range(2):
                    for ch in range(8):
                        mm(pb[4 + hf][:, :], mixT[:, ch, :], wout_bf[:, ch, hf * 512:(hf + 1) * 512], ch == 0, ch == 7,
                           ["mixT", "wout_bf"], [PB[4 + hf]])
                for hf in range(2):
                    hs_ = slice(hf * 512, (hf + 1) * 512)
                    stt("dve", u[:, hs_], xot[:, hs_], ALPHA, pb[4 + hf][:, :], ALU.mult, ALU.add, [xok, PB[4 + hf]], [uk + "_%d" % hf])
                s.op("dve", lambda e: e.engine_nop(), [uk + "_0", uk + "_1"], [uk])
                if xh_ is not None:
                    lc_cast(xh_, xojC[1 - sl][:, :, :], "xojC%d" % (1 - sl), "act")

            def stage2(j):
                jb = slice(j * 128, (j + 1) * 128)
                sl = j % 2
                u = uL[sl]
                uk = "u%d" % sl
                layer_norm(u, hs[:, :], lnt1, stats, mv, rstd, lntmp, uk, "hs", "l1")
                dma(hbuf[jb, :], hs[:, :], ["hs"], ["hbuf%d" % j])
                cp("act", h_bf[:, :], hs[:, :], ["hs"], ["h_bf"])
                for dc in range(8):
                    b = dc // 4
                    tr(pb[b][:, (dc % 4) * 128:(dc % 4 + 1) * 128], hs[:, dc * 128:(dc + 1) * 128], identf[:, :], ["hs", "identf"], [PB[b]])
                for bk in range(2):
                    cp("act" if bk == 0 else "dve", hT[:, 4 * bk:4 * bk + 4, :], pb[bk][:, :].rearrange("p (c q) -> p c q", c=4), [PB[bk]], ["hT%d" % bk])
                for dc in range(8):
                    mm(pb[2][:, 0:32], hT[:, dc, :], wr_sb[:, dc, :], dc == 0, dc == 7, ["hT0", "hT1", "wr_sb"], [PB[2]])
                tt("dve", lg[:, :], pb[2][:, 0:32], br_sb[:, :], ALU.add, [PB[2], "br_sb"], ["lg"])
                s.op("dve", lambda e: e.max(out=r8[:, :], in_=lg[:, :]), ["lg"], ["r8"])
                ts("dve", Mf[:, :], lg[:, :], r8[:, 3:4], ALU.is_ge, ["lg", "r8"], ["Mf"])
                cp("dve", Mb[:, :], Mf[:, :], ["Mf"], ["Mb"])
                ts("dve", sm1[:, :], r8[:, 0:1], -1.0, ALU.mult, ["r8"], ["sm1"])
                act(ex[:, :], lg[:, :], AF.Exp, ["lg", "sm1"], ["ex"], bias=sm1[:, 0:1])
                tt("dve", ex[:, :], ex[:, :], Mf[:, :], ALU.mult, ["ex", "Mf"], ["ex"])
                s.op("dve", lambda e: e.reduce_sum(sm1[:, :], ex[:, :], AX.X), ["ex", "sm1"], ["sm1"])
                s.op("dve", lambda e: e.reciprocal(sm1[:, :], sm1[:, :]), ["sm1"], ["sm1"])
                ts("dve", Gt[:, :], ex[:, :], sm1[:, 0:1], ALU.mult, ["ex", "sm1"], ["Gt"])
                mm(pb[2][:, 32:64], consts[:, C_TRIU, :], Mb[:, :], True, True, ["consts", "Mb"], [PB[2]])
                mm(pb[2][:, 64:96], consts[:, C_ONES, :], Mb[:, :], True, True, ["consts", "Mb"], [PB[2]])
                tt("dve", rank[:, :], pb[2][:, 32:64], base_bc[:, :], ALU.add, [PB[2], "base_bc"], ["rank"])
                tt("dve", base_bc[:, :], pb[2][:, 64:96], base_bc[:, :], ALU.add, [PB[2], "base_bc"], ["base_bc"])
                ts("dve", vld[:, :], rank[:, :], float(CAP), ALU.is_lt, ["rank"], ["vld"])
                tt("dve", vld[:, :], vld[:, :], Mf[:, :], ALU.mult, ["vld", "Mf"], ["vld"])
                tt("dve", slotm[:, :], rank[:, :], ecap[:, :], ALU.add, ["rank", "ecap"], ["slotm"])
                ts("dve", slotm[:, :], slotm[:, :], 1.0, ALU.add, ["slotm"], ["slotm"])
                tt("dve", slotm[:, :], slotm[:, :], vld[:, :], ALU.mult, ["slotm", "vld"], ["slotm"])
                ts("dve", slotm[:, :], slotm[:, :], -1.0, ALU.add, ["slotm"], ["slotm"])
                s.op("dve", lambda e: e.max(out=s8[:, :], in_=slotm[:, :]), ["slotm"], ["s8"])
                ts("dve", fix[:, :], s8[:, 0:4], 0.0, ALU.is_lt, ["s8"], ["fix"], float(NSLOT + 1), ALU.mult)
                tt("dve", fix[:, :], fix[:, :], s8[:, 0:4], ALU.add, ["fix", "s8"], ["fix"])
                ts("dve", fix[:, :], fix[:, :], 0.0, ALU.max, ["fix"], ["fix"], float(NSLOT), ALU.min)
                cp("dve", idx_all[:, j, :], fix[:, :], ["fix"], ["idx%d" % j])
                for k in range(4):
                    ts("dve", oh[:, :], slotm[:, :], s8[:, k:k + 1], ALU.is_equal, ["slotm", "s8"], ["oh"])
                    tt("dve", oh[:, :], oh[:, :], Gt[:, :], ALU.mult, ["oh", "Gt"], ["oh"])
                    s.op("dve", (lambda k_, j_: (lambda e: e.reduce_sum(gates_all[:, j_, k_:k_ + 1], oh[:, :], AX.X)))(k, j),
                         ["oh"], ["gates%d_%d" % (j, k)])
                    s.dma((lambda k_, j_: (lambda e: e.indirect_dma_start(
                        out=Xbuf, out_offset=bass.IndirectOffsetOnAxis(ap=idx_all[:, j_, k_:k_ + 1], axis=0),
                        in_=h_bf[:, :], in_offset=None)))(k, j),
                        ["idx%d" % j, "h_bf"], ["Xbuf_w%d_%d" % (j, k)], q="pool")
                tr(pb[2][0:32, 384:512], Gt[:, :], identf[:, :], ["Gt", "identf"], [PB[2]])
                cp("act", GT_all[:, j, :], pb[2][0:32, 384:512], [PB[2]], ["GT%d" % j])

            stage1(0)
            stage1b(0)
            for j in range(NJ):
                if j + 1 < NJ:
                    stage1(j + 1)
                stage2(j)
                if j + 1 < NJ:
                    stage1b(j + 1)
            s.emit()

        s.enabled = _LV >= 4
        xkeys = ["Xbuf_w%d_%d" % (j, k) for j in range(NJ) for k in range(4)]
        with contextlib.ExitStack() as ph:
            win_bf = [TT(ph, "win_bf%d" % i, [128, 8, 2048], BF16) for i in range(2)]
            wo_bf = [TT(ph, "wo_bf%d" % i, [128, 8, DM], BF16) for i in range(2)]
            bei = TT(ph, "bei", [128, 32, 16], F32)
            xrow = [TT(ph, "xrow%d" % i, [128, 3, DM], BF16) for i in range(2)]
            xeT = [TT(ph, "xeT%d" % i, [128, 8, CAP], BF16) for i in range(2)]
            AT = [TT(ph, "AT%d" % i, [128, 8, CAP], BF16) for i in range(2)]
            g1 = [TT(ph, "g1_%d" % i, [128, CAP], F32) for i in range(2)]
            sgt = [TT(ph, "sgt%d" % i, [128, CAP], F32) for i in range(2)]
            u1 = [TT(ph, "u1_%d" % i, [128, CAP], F32) for i in range(2)]
            ysb = [TT(ph, "ysb%d" % i, [128, DM], F32) for i in range(2)]
            dma(bei[:], beiD, [], ["bei"])
            for i in range(3, 5):
                stg.append(TT(ph, "stg%d" % i, [128, 2048], F32))
            nstg["n"] = 5
            NSTG = 5

            def expert_tasks(e):
                sl = e % 2
                wv = wei[e].rearrange("(dc p) f -> p dc f", p=128)
                ov = weo[e].rearrange("(fc p) c -> p fc c", p=128)
                cengs = ["act", "dve", "act", "dve", "act", "pool", "act", "dve", "act", "dve", "dve", "act"]
                tasks = []
                for dc in range(8):
                    tasks.append((win_bf[sl][:, dc, :], wv[:, dc, :], "win_bf%d_%d" % (sl, dc)))
                for i in range(4):
                    tasks.append((wo_bf[sl][:, 2 * i:2 * i + 2, :], ov[:, 2 * i:2 * i + 2, :], "wo_bf%d" % sl))
                return [(d, sr, k, cengs[i]) for i, (d, sr, k) in enumerate(tasks)]

            def task_dma(t):
                i = st_state["i"] % nstg["n"]
                st_state["i"] += 1
                sv = stg[i][:, 0:2048]
                if len(t[1].shape) == 3:
                    sv = sv.rearrange("p (a b) -> p a b", a=2)
                dma(sv, t[1], [], ["stg%d" % i])
                return (sv, i)

            def task_cast(t, h):
                cp(t[3], t[0], h[0], ["stg%d" % h[1]], [t[2]])

            def xrow_dma(e):
                sl = e % 2
                dma(xrow[sl][:, :, :], Xbuf[e * CAP:(e + 1) * CAP, :].rearrange("(st p) d -> p st d", p=128), xkeys, ["xrow%d" % sl])

            t0s = expert_tasks(0)
            xrow_dma(0)
            hs0 = {}
            for c in range(len(t0s)):
                if c < NSTG:
                    hs0[c] = task_dma(t0s[c])
            for c in range(len(t0s)):
                task_cast(t0s[c], hs0[c])
                if c + NSTG < len(t0s):
                    hs0[c + NSTG] = task_dma(t0s[c + NSTG])
            casts_per_ft = [2, 1, 2, 1, 2, 1, 2, 1]
            for e in range(32):
                sl = e % 2
                nxt = expert_tasks(e + 1) if e + 1 < 32 else []
                nh = {}
                nstate = {"c": 0}
                if nxt:
                    xrow_dma(e + 1)
                    for c in range(NSTG):
                        nh[c] = task_dma(nxt[c])

                def advance(n):
                    for _ in range(n):
                        c = nstate["c"]
                        if c >= len(nxt):
                            return
                        task_cast(nxt[c], nh[c])
                        if c + NSTG < len(nxt):
                            nh[c + NSTG] = task_dma(nxt[c + NSTG])
                        nstate["c"] += 1
                for st_ in range(3):
                    for dc in range(8):
                        tr(pb[6][:, :].bitcast(BF16)[:, dc * 128:(dc + 1) * 128], xrow[sl][:, st_, dc * 128:(dc + 1) * 128], ident_bf,
                           ["xrow%d" % sl, "consts"], [PB[6]])
                    cp("act" if st_ != 1 else "dve", xeT[sl][:, :, st_ * 128:(st_ + 1) * 128],
                       pb[6][:, :].bitcast(BF16).rearrange("p (c q) -> p c q", c=8), [PB[6]], ["xeT%d_%d" % (sl, st_)])
                xek = ["xeT%d_%d" % (sl, i) for i in range(3)]
                for ft in range(8):
                    bg, bu, w2 = ft % 2, 2 + ft % 2, ft % 2
                    for dc in range(8):
                        mm(pb[bg][:, 0:CAP], win_bf[sl][:, dc, ft * 128:(ft + 1) * 128], xeT[sl][:, dc, :], dc == 0, dc == 7,
                           ["win_bf%d_%d" % (sl, d_) for d_ in range(8)] + xek, [PB[bg]])
                    for dc in range(8):
                        mm(pb[bu][:, 0:CAP], win_bf[sl][:, dc, 1024 + ft * 128:1024 + (ft + 1) * 128], xeT[sl][:, dc, :], dc == 0, dc == 7,
                           ["win_bf%d_%d" % (sl, d_) for d_ in range(8)] + xek, [PB[bu]])
                    ts("dve", g1[w2][:, :], pb[bg][:, 0:CAP], bei[:, e, ft:ft + 1], ALU.add, [PB[bg], "bei"], ["g1_%d" % w2], 7.0, ALU.min)
                    act(sgt[w2][:, :], g1[w2][:, :], AF.Silu, ["g1_%d" % w2], ["sgt%d" % w2], scale=1.702)
                    ts("dve", u1[w2][:, :], pb[bu][:, 0:CAP], bei[:, e, 8 + ft:9 + ft], ALU.add, [PB[bu], "bei"], ["u1_%d" % w2], 7.0, ALU.min)
                    ts("dve", u1[w2][:, :], u1[w2][:, :], -7.0, ALU.max, ["u1_%d" % w2], ["u1_%d" % w2], 1.0, ALU.add)
                    stt("dve", AT[sl][:, ft, :], sgt[w2][:, :], 1.0 / 1.702, u1[w2][:, :], ALU.mult, ALU.mult,
                        ["sgt%d" % w2, "u1_%d" % w2], ["AT%d_%d" % (sl, ft)])
                    advance(casts_per_ft[ft])
                atk = ["AT%d_%d" % (sl, ft) for ft in range(8)]
                for st_ in range(3):
                    ys = (e * 3 + st_) % 2
                    for hf in range(2):
                        b = 4 + hf
                        for ft in range(8):
                            mm(pb[b][:, :], AT[sl][:, ft, st_ * 128:(st_ + 1) * 128], wo_bf[sl][:, ft, hf * 512:(hf + 1) * 512], ft == 0, ft == 7,
                               atk + ["wo_bf%d" % sl], [PB[b]])
                        cp("act", ysb[ys][:, hf * 512:(hf + 1) * 512], pb[b][:, :], [PB[b]], ["ysb%d_%d" % (ys, hf)])
                    dma(Ybuf[e * CAP + st_ * 128:e * CAP + (st_ + 1) * 128, :], ysb[ys][:, :], ["ysb%d_0" % ys, "ysb%d_1" % ys], ["Ybuf_%d_%d" % (e, st_)], q="act")
                advance(99)
            s.emit()

        nstg["n"] = 3
        s.enabled = _LV >= 5
        ykeys = ["Ybuf_%d_%d" % (e, st_) for e in range(32) for st_ in range(3)]
        with contextlib.ExitStack() as ph:
            yg = [[TT(ph, "yg%d_%d" % (i, k), [128, DM], F32) for k in range(4)] for i in range(2)]
            hh_ = [TT(ph, "hh%d" % i, [128, DM], F32) for i in range(2)]
            acc = [TT(ph, "acc%d" % i, [128, DM], F32) for i in range(2)]
            tmp = TT(ph, "tmpd", [128, DM], F32)
            ot = [TT(ph, "ot%d" % i, [128, DM], F32) for i in range(2)]
            stats = TT(ph, "stats2", [128, 2, 6], F32)
            mv = TT(ph, "mv2", [128, 2], F32)
            rstd = TT(ph, "rstd2", [128, 1], F32)
            beo_sb = TT(ph, "beo_sb", [32, DM], F32)
            lnt2 = TT(ph, "lnt2", [128, 2, DM], F32)
            dma(lnt2[:], lnD[:, 2:4, :], [], ["l2lnt"])
            dma(beo_sb[:, :], beoD, [], ["beo_sb"])
            for j in range(NJ):
                sl = j % 2
                jb = slice(j * 128, (j + 1) * 128)
                dma(hh_[sl][:, :], hbuf[jb, :], ["hbuf%d" % j], ["hh%d" % sl])
                for k in range(4):
                    s.dma((lambda k_, j_, sl_: (lambda e: e.indirect_dma_start(
                        out=yg[sl_][k_][:, :], out_offset=None, in_=Ybuf,
                        in_offset=bass.IndirectOffsetOnAxis(ap=idx_all[:, j_, k_:k_ + 1], axis=0))))(k, j, sl),
                        ykeys + ["idx%d" % j], ["yg%d_%d" % (sl, k)], q="pool")
                for hf in range(2):
                    mm(pb[hf][:, :], GT_all[:, j, :], beo_sb[:, hf * 512:(hf + 1) * 512], True, True, ["GT%d" % j, "beo_sb"], [PB[hf]])
                for hf in range(2):
                    hs_ = slice(hf * 512, (hf + 1) * 512)
                    stt("dve", acc[sl][:, hs_], hh_[sl][:, hs_], ALPHA, pb[hf][:, :], ALU.mult, ALU.add, ["hh%d" % sl, PB[hf]], ["acc%d_%d" % (sl, hf)])
                akeys = ["acc%d_0" % sl, "acc%d_1" % sl]
                for k in range(4):
                    eng = "dve"
                    stt(eng, acc[sl][:, :], yg[sl][k][:, :], gates_all[:, j, k:k + 1], acc[sl][:, :], ALU.mult, ALU.add,
                        ["yg%d_%d" % (sl, k), "gates%d_%d" % (j, k)] + akeys, akeys)
                s.op("dve", lambda e: e.engine_nop(), akeys, ["acc%d" % sl])
                layer_norm(acc[sl], ot[sl][:, :], lnt2, stats, mv, rstd, tmp, "acc%d" % sl, "ot%d" % sl, "l2")
                tok = dma(yout[jb, :], ot[sl][:, :], ["ot%d" % sl], ["yout%d" % j])
                if tok is not None:
                    s.out_toks.append(tok)
            s.emit(final=True)
    return nc


_BF = ml_dtypes.bfloat16


def _common_consts():
    key = np.arange(128)[:, None]
    q = np.arange(128)[None, :]
    causal = (key <= q).astype(np.float32)
    anti = (key > q).astype(np.float32)
    ones = np.ones((128, 128), np.float32)
    triu = (key < q).astype(np.float32)
    ident = np.eye(128, dtype=np.float32)
    cs = np.arange(512) - 1
    cstart = cs * 16
    sstart = np.arange(128) * 64
    ov = ((cstart[:, None] < sstart[None, :] + 64) & (cstart[:, None] + 32 > sstart[None, :]) & (cs[:, None] >= 0)).astype(np.float32)
    ovl = ov.reshape(4, 128, 128).transpose(1, 0, 2)
    Ex = (np.arange(S)[None, :] // 64 == np.arange(128)[:, None]).astype(np.float32)
    return causal, anti, ones, triu, ident, ovl, Ex


def _rope_tab(pos):
    half = 32
    inv = (np.float32(10000.0) ** (-np.arange(half, dtype=np.float32) / np.float32(half))).astype(np.float32)
    ang = pos.astype(np.float32)[:, None] * inv[None, :]
    cos = np.cos(ang).astype(np.float32).T
    sin = np.sin(ang).astype(np.float32).T
    p = np.arange(128)
    c = cos[p % 32]
    sgn = np.where((p % 64) < 32, -1.0, 1.0).astype(np.float32)[:, None]
    sS = sin[p % 32] * sgn
    return np.stack([c, sS], 1).astype(np.float32)


def kernel(x, w_in, nsa_k_pe, nsa_k_w1, nsa_k_w2, nsa_v_pe, nsa_v_w1, nsa_v_w2, swa_sinks,
           w_br_nsa, w_br_swa, w_out, ln1_g, ln1_b, w_router, b_router, w_expert_in, b_expert_in,
           w_expert_out, b_expert_out, ln2_g, ln2_b):
    f = lambda a: np.ascontiguousarray(np.asarray(a, dtype=np.float32))
    x = f(x); w = f(w_in)[0]
    causal, anti, ones, triu, ident, ovl, Ex = _common_consts()
    qn, kc, vc, ks, vs, kw, vw, gn, qs, ksw, vsw, gm = np.split(w, np.cumsum(
        [512, 128, 128, 128, 128, 128, 128, 24, 512, 128, 128])[:], axis=1)

    def qperm(m):
        hs = m.reshape(DM, 8, 64)
        return np.concatenate([np.concatenate([hs[:, hh], hs[:, 4 + hh]], 1) for hh in range(4)], 1)

    shared = {
        "w_kvf": f(np.concatenate([kc, vc, ks, kw, ksw], 1)),
        "w_vt": f(np.concatenate([vs, vw, vsw], 1)),
        "w_q": f(np.concatenate([qperm(qn), qperm(qs)], 1)),
        "w_gn": f(gn), "w_gm": f(gm),
        "cw1": f(np.stack([f(nsa_k_w1)[0], f(nsa_v_w1)[0]], 0)),
        "cw2": f(np.stack([f(nsa_k_w2)[0], f(nsa_v_w2)[0]], 1)),
        "ovl": ovl.astype(_BF), "ind": ((np.arange(S)[None, :] // 64) % 64 == np.arange(64)[:, None]).astype(np.float32).astype(_BF), "identf": ident,
        "sinks": f(np.broadcast_to(f(swa_sinks)[0][None, :], (128, 8))),
        "wbr": f(np.stack([f(w_br_nsa)[0], f(w_br_swa)[0]], 0)), "wout": f(w_out)[0],
        "ln": f(np.broadcast_to(np.stack([f(ln1_g)[0], f(ln1_b)[0], f(ln2_g)[0], f(ln2_b)[0]], 0)[None], (128, 4, DM))),
        "wr": f(w_router)[0], "br": f(np.broadcast_to(f(b_router)[0][None, :], (128, 32))),
        "ecap": f(np.broadcast_to((np.arange(32, dtype=np.float32) * CAP)[None, :], (128, 32))),
        "wei": f(w_expert_in)[0], "weo": f(w_expert_out)[0], "beo": f(b_expert_out)[0],
        "bei": f(f(b_expert_in)[0].reshape(32, 16, 128).transpose(2, 0, 1)),
    }
    pes = []
    for pe in (f(nsa_k_pe)[0], f(nsa_v_pe)[0]):
        pes.append(pe.reshape(16, 2, 64).transpose(1, 2, 0).reshape(128, 16))
    shared["pe2"] = f(np.stack(pes, 1))

    in_maps = []
    for c in range(8):
        b, r = c // 4, c % 4
        pad = (3 - r) * 128
        xT = x[b].T
        xTf = np.zeros((DM, S), np.float32)
        xTf[:, pad:] = xT[:, :S - pad]
        own = np.concatenate([np.arange((4 * j + r) * 128, (4 * j + r + 1) * 128) for j in range(NJ)])
        m = dict(shared)
        m["xTf"] = xTf
        m["xTo"] = f(xT[:, own])
        m["xo"] = f(x[b][own])
        m["ropef"] = _rope_tab(np.maximum(np.arange(S) - pad, 0))
        ro = _rope_tab(own)
        ro = ro.reshape(128, 2, NJ, 1, 128).transpose(0, 2, 1, 3, 4)
        m["ropeo"] = f(np.broadcast_to(ro, (128, NJ, 2, 4, 128)).reshape(128, NJ, 2, 512))
        cidx = np.arange(512)
        cs_real_start = 16 * (cidx - 1) - pad
        cvalid = (cidx >= 1) & (cs_real_start >= 0)
        cm = cvalid[:, None] & ((cs_real_start + 31)[:, None] <= own[None, :])
        cm = cm.reshape(4, 128, NJ, 1, 128).transpose(1, 2, 0, 3, 4)
        cmb = np.where(np.broadcast_to(cm, (128, NJ, 4, 4, 128)), 0.0, -30000.0).astype(np.float32)
        m["cmaskT"] = np.ascontiguousarray(cmb.reshape(128, NJ, 4, 512)).astype(_BF)
        jr = np.arange(128) - 2 * (3 - r)
        cur = own // 64
        jrr = jr[None, :]
        fut = jrr > cur[:, None]
        padb = jrr < 0
        forced = (jrr == 0) | (jrr == cur[:, None]) | (jrr == cur[:, None] - 1)
        A = np.where(fut | padb | forced, 0.0, 1.0).astype(np.float32)
        Bm = np.where(fut | padb, -1.0, np.where(forced, 1e6, 0.0)).astype(np.float32)
        ABm = np.stack([A, Bm], 1).reshape(NJ, 128, 2, 128).transpose(1, 0, 2, 3)
        m["AB"] = f(ABm)
        m["validm"] = f(np.broadcast_to((jr >= 0).astype(np.float32)[None, :], (128, 128)))
        w0 = ones * float(0 >= 3 - r); w1 = ones * float(1 >= 3 - r); w2 = ones * float(2 >= 3 - r)
        s2 = anti * float(r >= 1)
        m["consts"] = np.stack([causal, anti, w0, w1, w2, s2, triu, ones, ident], 1).astype(_BF)
        mb = [np.where(np.tile(mk_, (1, 4)) > 0.5, 0.0, -30000.0).astype(np.float32) for mk_ in (causal, anti, w0, w1, w2, s2)]
        m["mb4"] = np.stack(mb, 1).astype(_BF)
        in_maps.append(m)

    nc = build_nc()
    res = run_bass_kernel_spmd(nc, in_maps, core_ids=list(range(8)))
    out = np.zeros((2, S, DM), np.float32)
    for c in range(8):
        b, r = c // 4, c % 4
        y = np.asarray(res.results[c]["y"], dtype=np.float32)
        for j in range(NJ):
            out[b, (4 * j + r) * 128:(4 * j + r + 1) * 128] = y[j * 128:(j + 1) * 128]
    return out
```
